# Optimizing a Trainium2 kernel written in Bass

```python
import numpy as np
import jax
import jax.numpy as jnp
from jax import lax

D_MODEL = 2048
BATCH = 4
SEQ = 4096
DEPTH = 2

HEAD_DIM = 64
SWA_HEADS = D_MODEL // HEAD_DIM // 2
SWA_KV_HEADS = 2
SWA_WINDOW = 128
NSA_HEADS = D_MODEL // HEAD_DIM - SWA_HEADS
NSA_KV_GROUPS = 2
NSA_CMP_LEN = 32
NSA_CMP_STRIDE = 16
NSA_CMP_HIDDEN = 256
NSA_SLC_LEN = 64
NSA_TOPK = 16
NSA_WINDOW = 512
Q_BLOCK = 128
NSA_Q_CHUNK = 64
N_GROUPS = 8
EXPERTS_PER_GROUP = 8
N_EXPERTS = N_GROUPS * EXPERTS_PER_GROUP
EXPERT_TOPK = 2
EXPERT_HIDDEN = 512
EXPERT_ROWS = 128
SWA_Q_W = SWA_HEADS * HEAD_DIM
SWA_KV_W = SWA_KV_HEADS * HEAD_DIM
NSA_Q_W = NSA_HEADS * HEAD_DIM
NSA_KV_W = NSA_KV_GROUPS * HEAD_DIM
NSA_GATE_W = 3 * NSA_HEADS
IN_COLS = SWA_Q_W + 2 * SWA_KV_W + NSA_Q_W + 6 * NSA_KV_W + NSA_GATE_W
MIX_WIDTH = SWA_Q_W + NSA_Q_W
ALPHA = (2.0 * DEPTH) ** 0.25
BETA = (8.0 * DEPTH) ** -0.25
LN_EPS = 1e-5

kernel_name = 'hybrid_swa_nsa_hier_moe'


def layer_norm(x, g, b):
    xf = x.astype(jnp.float32)
    mu = jnp.mean(xf, axis=-1, keepdims=True)
    var = jnp.mean(jnp.square(xf - mu), axis=-1, keepdims=True)
    return ((xf - mu) * lax.rsqrt(var + LN_EPS) * g + b).astype(x.dtype)


def alibi_slopes():
    n = SWA_HEADS + NSA_HEADS
    s = jnp.exp2(-8.0 * jnp.arange(1, n + 1, dtype=jnp.float32) / n)
    return s[0::2], s[1::2]


def banded_attention(q, k, v, slopes, window, sinks=None):
    B, S, G, R, Dh = q.shape
    nb = S // Q_BLOCK
    span = window + Q_BLOCK
    kp = jnp.pad(k, ((0, 0), (window, 0), (0, 0), (0, 0)))
    vp = jnp.pad(v, ((0, 0), (window, 0), (0, 0), (0, 0)))
    qb = q.reshape(B, nb, Q_BLOCK, G, R, Dh).swapaxes(0, 1)
    scale = Dh ** -0.5
    slope = slopes.astype(jnp.float32)[None, :, :, None, None]

    def block(args):
        j, qj = args
        kj = lax.dynamic_slice_in_dim(kp, j * Q_BLOCK, span, axis=1)
        vj = lax.dynamic_slice_in_dim(vp, j * Q_BLOCK, span, axis=1)
        t = j * Q_BLOCK + jnp.arange(Q_BLOCK)
        s = j * Q_BLOCK - window + jnp.arange(span)
        dist = t[:, None] - s[None, :]
        mask = (dist >= 0) & (dist < window) & (s[None, :] >= 0)
        logits = jnp.einsum('bqgrd,bkgd->bgrqk', qj, kj).astype(jnp.float32) * scale
        logits = logits - slope * dist.astype(jnp.float32)
        logits = jnp.where(mask, logits, -jnp.inf)
        m = jnp.max(logits, axis=-1, keepdims=True)
        if sinks is None:
            p = jnp.exp(logits - m)
            denom = jnp.sum(p, axis=-1, keepdims=True)
        else:
            sink = sinks.astype(jnp.float32)[None, :, :, None, None]
            m = jnp.maximum(m, sink)
            p = jnp.exp(logits - m)
            denom = jnp.sum(p, axis=-1, keepdims=True) + jnp.exp(sink - m)
        p = (p / denom).astype(vj.dtype)
        return jnp.einsum('bgrqk,bkgd->bqgrd', p, vj)

    out = lax.map(block, (jnp.arange(nb), qb))
    return out.swapaxes(0, 1).reshape(B, S, G, R, Dh)


def compress_blocks(kv, pe, w1, w2):
    B, S, G, Dh = kv.shape
    n_cmp = (S - NSA_CMP_LEN) // NSA_CMP_STRIDE + 1
    idx = jnp.arange(n_cmp)[:, None] * NSA_CMP_STRIDE + jnp.arange(NSA_CMP_LEN)[None, :]
    blocks = kv[:, idx] + pe[None, None, :, None, :]
    flat = blocks.transpose(0, 1, 3, 2, 4).reshape(B, n_cmp, G, NSA_CMP_LEN * Dh)
    return jax.nn.gelu(flat @ w1) @ w2


def nsa_compressed_selected(q, k_cmp, v_cmp, k_slc, v_slc, slopes):
    B, S, G, R, Dh = q.shape
    n_cmp = k_cmp.shape[1]
    n_slc = S // NSA_SLC_LEN
    k_sel = min(NSA_TOPK, n_slc)
    nc = S // NSA_Q_CHUNK
    scale = Dh ** -0.5
    slope = slopes.astype(jnp.float32)
    cmp_start = jnp.arange(n_cmp) * NSA_CMP_STRIDE
    cmp_end = cmp_start + NSA_CMP_LEN - 1
    slc_start = jnp.arange(n_slc) * NSA_SLC_LEN
    overlap = ((cmp_start[:, None] < slc_start[None, :] + NSA_SLC_LEN)
               & (cmp_end[:, None] >= slc_start[None, :])).astype(jnp.float32)
    ks_blocks = k_slc.reshape(B, n_slc, NSA_SLC_LEN, G, Dh).transpose(0, 3, 1, 2, 4)
    vs_blocks = v_slc.reshape(B, n_slc, NSA_SLC_LEN, G, Dh).transpose(0, 3, 1, 2, 4)
    bi = jnp.arange(B)[:, None, None, None]
    gi = jnp.arange(G)[None, :, None, None]
    blk = jnp.arange(n_slc)
    qc = q.reshape(B, nc, NSA_Q_CHUNK, G, R, Dh).swapaxes(0, 1)

    def chunk(args):
        c, qj = args
        t = c * NSA_Q_CHUNK + jnp.arange(NSA_Q_CHUNK)
        dist_c = (t[:, None] - cmp_end[None, :]).astype(jnp.float32)
        lc = jnp.einsum('bqgrd,bngd->bgrqn', qj, k_cmp).astype(jnp.float32) * scale
        lc = lc - slope[None, :, :, None, None] * dist_c
        lc = jnp.where(dist_c >= 0, lc, -jnp.inf)
        mc = jnp.max(lc, axis=-1, keepdims=True)
        mc = jnp.where(jnp.isfinite(mc), mc, 0.0)
        pc = jnp.exp(lc - mc)
        dc = jnp.sum(pc, axis=-1, keepdims=True)
        pc = pc / jnp.where(dc > 0, dc, 1.0)
        o_cmp = jnp.einsum('bgrqn,bngd->bqgrd', pc.astype(v_cmp.dtype), v_cmp)
        imp = jnp.einsum('bgrqn,nj->bgqj', pc, overlap)
        cur = t // NSA_SLC_LEN
        valid = slc_start[None, :] <= t[:, None]
        forced = (blk[None, :] == 0) | (blk[None, :] == cur[:, None]) | (blk[None, :] == cur[:, None] - 1)
        score = jnp.where(forced, jnp.inf, imp)
        score = jnp.where(valid, score, -jnp.inf)
        top_s, top_i = lax.top_k(score, k_sel)
        kg = ks_blocks[bi, gi, top_i]
        vg = vs_blocks[bi, gi, top_i]
        pos = top_i[..., None] * NSA_SLC_LEN + jnp.arange(NSA_SLC_LEN)
        dist_s = t[None, None, :, None, None] - pos
        ok = (top_s > -jnp.inf)[..., None] & (dist_s >= 0)
        ls = jnp.einsum('bqgrd,bgqkld->bgrqkl', qj, kg).astype(jnp.float32) * scale
        ls = ls - slope[None, :, :, None, None, None] * dist_s.astype(jnp.float32)[:, :, None]
        ls = jnp.where(ok[:, :, None], ls, -jnp.inf)
        ps = jax.nn.softmax(ls.reshape(B, G, R, NSA_Q_CHUNK, k_sel * NSA_SLC_LEN), axis=-1)
        ps = ps.reshape(B, G, R, NSA_Q_CHUNK, k_sel, NSA_SLC_LEN).astype(vg.dtype)
        o_slc = jnp.einsum('bgrqkl,bgqkld->bqgrd', ps, vg)
        return o_cmp, o_slc

    o_cmp, o_slc = lax.map(chunk, (jnp.arange(nc), qc))
    o_cmp = o_cmp.swapaxes(0, 1).reshape(B, S, G, R, Dh)
    o_slc = o_slc.swapaxes(0, 1).reshape(B, S, G, R, Dh)
    return o_cmp, o_slc


def hybrid_mixer(x, w_in, b_in, sinks, pe_k, w1_k, w2_k, pe_v, w1_v, w2_v, w_out, b_out):
    B, S, _ = x.shape
    Dh = HEAD_DIM
    proj = x @ w_in + b_in
    sizes = [SWA_Q_W, SWA_KV_W, SWA_KV_W, NSA_Q_W, 6 * NSA_KV_W, NSA_GATE_W]
    offs = []
    acc = 0
    for n in sizes[:-1]:
        acc += n
        offs.append(acc)
    qa, ka, va, qn, kvn, gn = jnp.split(proj, offs, axis=-1)
    slopes_a, slopes_n = alibi_slopes()
    ra = SWA_HEADS // SWA_KV_HEADS
    qa = qa.reshape(B, S, SWA_KV_HEADS, ra, Dh)
    ka = ka.reshape(B, S, SWA_KV_HEADS, Dh)
    va = va.reshape(B, S, SWA_KV_HEADS, Dh)
    o_a = banded_attention(qa, ka, va, slopes_a.reshape(SWA_KV_HEADS, ra), SWA_WINDOW,
                           sinks.reshape(SWA_KV_HEADS, ra))
    rn = NSA_HEADS // NSA_KV_GROUPS
    qn = qn.reshape(B, S, NSA_KV_GROUPS, rn, Dh)
    kvn = kvn.reshape(B, S, 6, NSA_KV_GROUPS, Dh)
    k_cmp = compress_blocks(kvn[:, :, 0], pe_k, w1_k, w2_k)
    v_cmp = compress_blocks(kvn[:, :, 1], pe_v, w1_v, w2_v)
    sl_n = slopes_n.reshape(NSA_KV_GROUPS, rn)
    o_cmp, o_slc = nsa_compressed_selected(qn, k_cmp, v_cmp, kvn[:, :, 2], kvn[:, :, 3], sl_n)
    o_win = banded_attention(qn, kvn[:, :, 4], kvn[:, :, 5], sl_n, NSA_WINDOW)
    g = jax.nn.sigmoid(gn).reshape(B, S, NSA_KV_GROUPS, rn, 3, 1)
    o_n = g[..., 0, :] * o_cmp + g[..., 1, :] * o_slc + g[..., 2, :] * o_win
    o = jnp.concatenate([o_a.reshape(B, S, SWA_Q_W), o_n.reshape(B, S, NSA_Q_W)], axis=-1)
    return o @ w_out + b_out


def hier_moe(x, w_group, b_group, w_expert, b_expert, we_gate, we_up, we_down):
    B, S, D = x.shape
    N = B * S
    T = EXPERT_ROWS
    xf = x.reshape(N, D)
    g_logits = (xf @ w_group + b_group).astype(jnp.float32)
    g_prob = jax.nn.softmax(g_logits, axis=-1)
    g_sel = jnp.argmax(g_logits, axis=-1)
    g_w = jnp.take_along_axis(g_prob, g_sel[:, None], axis=-1)
    e_logits = (xf @ w_expert + b_expert).astype(jnp.float32).reshape(N, N_GROUPS, EXPERTS_PER_GROUP)
    e_logits = jnp.take_along_axis(e_logits, g_sel[:, None, None], axis=1)[:, 0]
    top_v, top_local = lax.top_k(e_logits, EXPERT_TOPK)
    e_w = jax.nn.softmax(top_v, axis=-1) * g_w
    eid = (g_sel[:, None] * EXPERTS_PER_GROUP + top_local).reshape(-1)
    wts = e_w.reshape(-1)
    tok = jnp.repeat(jnp.arange(N, dtype=jnp.int32), EXPERT_TOPK)
    A = N * EXPERT_TOPK
    order = jnp.argsort(eid)
    eid_s, tok_s, w_s = eid[order], tok[order], wts[order]
    counts = jnp.bincount(eid, length=N_EXPERTS)
    padded = (counts + T - 1) // T * T
    start = jnp.cumsum(counts) - counts
    pend = jnp.cumsum(padded)
    pstart = pend - padded
    dest = pstart[eid_s] + (jnp.arange(A) - start[eid_s])
    P = A + N_EXPERTS * T
    nblk = P // T
    row_tok = jnp.full((P,), N, dtype=jnp.int32).at[dest].set(tok_s)
    x_pad = jnp.concatenate([xf, jnp.zeros((1, D), xf.dtype)], axis=0)
    xb = x_pad[row_tok].reshape(nblk, T, D)
    blk_e = jnp.minimum(jnp.searchsorted(pend, jnp.arange(nblk) * T, side='right'), N_EXPERTS - 1)

    def run(args):
        e, xr = args
        h = jax.nn.silu(xr @ we_gate[e]) * (xr @ we_up[e])
        return h @ we_down[e]

    yb = lax.map(run, (blk_e, xb)).reshape(P, D)
    y = jax.ops.segment_sum(yb[dest] * w_s[:, None].astype(yb.dtype), tok_s, num_segments=N)
    return y.reshape(B, S, D)


def setup_inputs(seed: int = 0) -> dict:
    key = jax.random.key(seed)
    ks = jax.random.split(key, 24)
    D, L, Dh = D_MODEL, DEPTH, HEAD_DIM
    nrm = jax.random.normal
    segs = [(SWA_Q_W, 1.0), (SWA_KV_W, 1.0), (SWA_KV_W, BETA), (NSA_Q_W, 1.0)]
    segs += [(NSA_KV_W, 1.0 if i % 2 == 0 else BETA) for i in range(6)]
    segs += [(NSA_GATE_W, 1.0)]
    col_scale = jnp.asarray(np.concatenate([np.full(n, s, np.float32) for n, s in segs]))
    flat_in = NSA_CMP_LEN * Dh
    return {
        'x': nrm(ks[0], (BATCH, SEQ, D), jnp.float32),
        'w_in': nrm(ks[1], (L, D, IN_COLS), jnp.float32) * D ** -0.5 * col_scale,
        'b_in': nrm(ks[2], (L, IN_COLS), jnp.float32) * 0.02,
        'swa_sinks': nrm(ks[3], (L, SWA_HEADS), jnp.float32) * 0.5,
        'cmp_pe_k': nrm(ks[4], (L, NSA_CMP_LEN, Dh), jnp.float32) * 0.5,
        'cmp_w1_k': nrm(ks[5], (L, flat_in, NSA_CMP_HIDDEN), jnp.float32) * flat_in ** -0.5,
        'cmp_w2_k': nrm(ks[6], (L, NSA_CMP_HIDDEN, Dh), jnp.float32) * NSA_CMP_HIDDEN ** -0.5,
        'cmp_pe_v': nrm(ks[7], (L, NSA_CMP_LEN, Dh), jnp.float32) * 0.5,
        'cmp_w1_v': nrm(ks[8], (L, flat_in, NSA_CMP_HIDDEN), jnp.float32) * flat_in ** -0.5,
        'cmp_w2_v': nrm(ks[9], (L, NSA_CMP_HIDDEN, Dh), jnp.float32) * NSA_CMP_HIDDEN ** -0.5,
        'w_out': nrm(ks[10], (L, MIX_WIDTH, D), jnp.float32) * MIX_WIDTH ** -0.5 * BETA,
        'b_out': nrm(ks[11], (L, D), jnp.float32) * 0.02,
        'ln1_g': 1.0 + 0.05 * nrm(ks[12], (L, D), jnp.float32),
        'ln1_b': 0.02 * nrm(ks[13], (L, D), jnp.float32),
        'w_group': nrm(ks[14], (L, D, N_GROUPS), jnp.float32) * D ** -0.5,
        'b_group': nrm(ks[15], (L, N_GROUPS), jnp.float32) * 0.01,
        'w_expert': nrm(ks[16], (L, D, N_EXPERTS), jnp.float32) * D ** -0.5,
        'b_expert': nrm(ks[17], (L, N_EXPERTS), jnp.float32) * 0.01,
        'we_gate': nrm(ks[18], (L, N_EXPERTS, D, EXPERT_HIDDEN), jnp.float32) * D ** -0.5,
        'we_up': nrm(ks[19], (L, N_EXPERTS, D, EXPERT_HIDDEN), jnp.float32) * D ** -0.5,
        'we_down': nrm(ks[20], (L, N_EXPERTS, EXPERT_HIDDEN, D), jnp.float32) * EXPERT_HIDDEN ** -0.5 * BETA,
        'ln2_g': 1.0 + 0.05 * nrm(ks[21], (L, D), jnp.float32),
        'ln2_b': 0.02 * nrm(ks[22], (L, D), jnp.float32),
    }


def reference(x, w_in, b_in, swa_sinks, cmp_pe_k, cmp_w1_k, cmp_w2_k, cmp_pe_v, cmp_w1_v,
              cmp_w2_v, w_out, b_out, ln1_g, ln1_b, w_group, b_group, w_expert, b_expert,
              we_gate, we_up, we_down, ln2_g, ln2_b):
    for l in range(DEPTH):
        mix = hybrid_mixer(x, w_in[l], b_in[l], swa_sinks[l], cmp_pe_k[l], cmp_w1_k[l], cmp_w2_k[l],
                           cmp_pe_v[l], cmp_w1_v[l], cmp_w2_v[l], w_out[l], b_out[l])
        x = layer_norm(ALPHA * x + mix, ln1_g[l], ln1_b[l])
        ffn = hier_moe(x, w_group[l], b_group[l], w_expert[l], b_expert[l],
                       we_gate[l], we_up[l], we_down[l])
        x = layer_norm(ALPHA * x + ffn, ln2_g[l], ln2_b[l])
    return x
```

```python
import contextlib
import numpy as np
import ml_dtypes
import concourse.bass as bass
import concourse.mybir as mybir
from concourse.bass_utils import run_bass_kernel_spmd

F32 = mybir.dt.float32
BF16 = mybir.dt.bfloat16
I32 = mybir.dt.int32
U32 = mybir.dt.uint32
AF = mybir.ActivationFunctionType
ALU = mybir.AluOpType
AX = mybir.AxisListType
NEG = -30000.0


class Buf:
    __slots__ = ("name", "w", "r", "f")

    def __init__(self, name=""):
        self.name = name
        self.w = {}
        self.f = {}
        self.r = {}


class FW:
    EPOCH = 12000
    NDMA = 20

    def __init__(self, nc, es):
        self.nc = nc
        self.es = es
        self.eng = {"pe": nc.tensor, "act": nc.scalar, "dve": nc.vector, "pool": nc.gpsimd, "sp": nc.sync}
        self.cnt = {e: 0 for e in self.eng}
        self.csem = {}
        self.nsem = 0
        for e in ("pe", "act", "dve", "pool"):
            self.csem[e] = self._newsem(f"c_{e}")
        self.dsem = {}
        self.dcnt = {}
        for q in ("sp", "pool", "act"):
            self.dsem[q] = [self._newsem(f"d_{q}{i}") for i in range(self.NDMA)]
            self.dcnt[q] = 0
        self.known = {e: {} for e in self.eng}
        self.all_tokens = {}
        self.ninstr = 0

    def _newsem(self, name):
        self.nsem += 1
        return self.es.enter_context(self.nc.semaphore(f"{name}_{self.nsem}"))

    def _wait(self, e, tok):
        sem, val = tok
        k = id(sem)
        if self.known[e].get(k, 0) >= val:
            return
        self.eng[e].wait_ge(sem, val)
        self.known[e][k] = val

    def _deps(self, e, reads, writes, pwrites):
        for b in reads:
            for tok in b.w.values():
                self._wait(e, tok)
        for b in writes:
            for tok in b.w.values():
                self._wait(e, tok)
            for tok in b.r.values():
                self._wait(e, tok)
        for b in pwrites:
            for tok in b.f.values():
                self._wait(e, tok)
            for tok in b.r.values():
                self._wait(e, tok)

    def _record(self, tok, reads, writes, pwrites):
        sem, val = tok
        k = id(sem)
        self.all_tokens[k] = tok
        for b in reads:
            b.r[k] = tok
        for b in writes:
            b.w = {k: tok}
            b.f = {k: tok}
            b.r = {}
        for b in pwrites:
            b.w[k] = tok

    def op(self, e, fn, reads=(), writes=(), pwrites=()):
        self._deps(e, reads, writes, pwrites)
        if self.cnt[e] >= self.EPOCH:
            self.csem[e] = self._newsem(f"c_{e}")
            self.cnt[e] = 0
        ins = fn(self.eng[e])
        self.cnt[e] += 1
        sem = self.csem[e]
        ins.then_inc(sem, 1)
        tok = (sem, self.cnt[e])
        if e == "pe":
            self.known[e][id(sem)] = self.cnt[e]
        self._record(tok, reads, writes, pwrites)
        self.ninstr += 1
        return tok

    def dma(self, q, fn, reads=(), writes=(), pwrites=()):
        self._deps(q, reads, writes, pwrites)
        i = self.dcnt[q]
        sem = self.dsem[q][i % self.NDMA]
        rnd = i // self.NDMA
        if rnd > 0:
            self._wait(q, (sem, 16 * rnd))
        ins = fn(self.eng[q])
        ins.then_inc(sem, 16)
        self.dcnt[q] += 1
        tok = (sem, 16 * (rnd + 1))
        self._record(tok, reads, writes, pwrites)
        self.ninstr += 1
        return tok

    def barrier(self):
        for e in self.eng:
            for tok in list(self.all_tokens.values()):
                self._wait(e, tok)


class Cfg:
    def __init__(self, S=4096, DEPTH=2, NG=8, CAP=256, debug=False):
        self.S = S
        self.D = 2048
        self.DEPTH = DEPTH
        self.NG = NG
        self.EPG = 8
        self.NE = NG * 8
        self.CAP = CAP
        self.HID = 512
        self.NT = S // 128
        self.NQB = S // 512
        self.NCMP = (S - 32) // 16 + 1
        self.NSLC = S // 64
        self.KSEL = min(16, self.NSLC)
        self.ALPHA = (2.0 * 2) ** 0.25
        self.debug = debug
        self.branches = (0, 1, 2)
        self.look = 2
        self.c1skew = True


SRC_COLS = [i * 128 for i in range(8)] + [1280 + i * 128 for i in range(8)] + [1024, 2304, 2432, 2560, 2816]
NCHUNK = len(SRC_COLS)
ROW_KA, ROW_KC, ROW_VC, ROW_KS, ROW_KW = 2048, 2176, 2304, 2432, 2560


def _bf(x):
    return np.asarray(x, np.float32).astype(ml_dtypes.bfloat16)


def host_consts(cfg):
    S, NT, NCMP, NSLC = cfg.S, cfg.NT, cfg.NCMP, cfg.NSLC
    c = {}
    pos = np.arange(S)
    kaug = np.stack([pos // 64, pos % 64, np.ones(S), np.ones(S), np.ones(S)]).astype(np.float32)
    c["kaug"] = _bf(kaug)
    posc = 16 * np.arange(256) + 31
    kaugc = np.stack([posc // 64, posc % 64, np.ones(256), np.ones(256), np.ones(256)]).astype(np.float32)
    c["kaugc"] = _bf(kaugc)
    n = 32
    s_all = np.exp2(-8.0 * np.arange(1, n + 1, dtype=np.float32) / n).astype(np.float32)
    slopes = np.concatenate([s_all[0::2], s_all[1::2]])
    qaug = np.zeros((5, 32, S), np.float32)
    for h in range(32):
        ch = float(_bf(np.float32(8.0 * slopes[h])).astype(np.float32))
        A = -(np.float64(ch) * pos.astype(np.float64))
        a1 = _bf(A).astype(np.float64)
        a2 = _bf(A - a1).astype(np.float64)
        a3 = _bf(A - a1 - a2).astype(np.float64)
        qaug[0, h] = 64.0 * ch
        qaug[1, h] = ch
        qaug[2, h] = a1
        qaug[3, h] = a2
        qaug[4, h] = a3
    c["qaug"] = _bf(qaug)
    k = np.arange(128)[:, None]
    q = np.arange(128)[None, :]
    c["caus"] = _bf(np.where(k <= q, 0.0, NEG))
    c["anti"] = _bf(np.where(k > q, 0.0, NEG))
    q2 = np.arange(256)[None, :]
    c["swam"] = _bf(np.where((q2 - k >= 0) & (q2 - k < 128), 0.0, NEG))
    W = 8 * (NT - 1) + NCMP
    m = np.arange(W)[None, :]
    p = np.arange(128)[:, None]
    c["cmpm"] = _bf(np.where(16 * (m - 8 * (NT - 1)) + 31 <= p, 0.0, NEG))
    E = (np.arange(S)[None, :] // 64 == np.arange(NSLC)[:, None]).astype(np.float32)
    c["eblk"] = _bf(E)
    t = np.arange(S)
    cur = t // 64
    j = np.arange(NSLC)[None, :]
    forced = (j == 0) | (j == cur[:, None]) | (j == cur[:, None] - 1)
    valid = (j * 64) <= t[:, None]
    sb = np.where(valid, np.where(forced, 1e9, 0.0), -1e9).astype(np.float32)
    c["selbias"] = np.ascontiguousarray(sb.reshape(NT, 128, NSLC).transpose(1, 0, 2))
    c["identb"] = _bf(np.eye(128))
    c["identf"] = np.eye(128, dtype=np.float32)
    c["ltri"] = (np.arange(128)[:, None] < np.arange(128)[None, :]).astype(np.float32)
    c["onesf"] = np.ones((128, 128), np.float32)
    c["iotae"] = np.tile(np.arange(cfg.NE, dtype=np.float32)[None, :], (128, 1))
    return c


CONST_DT = {"kaug": BF16, "kaugc": BF16, "qaug": BF16, "caus": BF16, "anti": BF16, "swam": BF16, "cmpm": BF16,
            "eblk": BF16, "selbias": F32, "identb": BF16, "identf": F32, "ltri": F32, "onesf": F32, "iotae": F32}

WEIGHT_SHAPES = lambda cfg: {
    "w_in": [cfg.DEPTH, 2048, 3120], "b_in": [cfg.DEPTH, 3120], "swa_sinks": [cfg.DEPTH, 16],
    "cmp_pe_k": [cfg.DEPTH, 32, 64], "cmp_w1_k": [cfg.DEPTH, 2048, 256], "cmp_w2_k": [cfg.DEPTH, 256, 64],
    "cmp_pe_v": [cfg.DEPTH, 32, 64], "cmp_w1_v": [cfg.DEPTH, 2048, 256], "cmp_w2_v": [cfg.DEPTH, 256, 64],
    "w_out": [cfg.DEPTH, 2048, 2048], "b_out": [cfg.DEPTH, 2048], "ln1_g": [cfg.DEPTH, 2048], "ln1_b": [cfg.DEPTH, 2048],
    "w_group": [cfg.DEPTH, 2048, cfg.NG], "b_group": [cfg.DEPTH, cfg.NG],
    "w_expert": [cfg.DEPTH, 2048, cfg.NE], "b_expert": [cfg.DEPTH, cfg.NE],
    "we_gate": [cfg.DEPTH, cfg.NE, 2048, 512], "we_up": [cfg.DEPTH, cfg.NE, 2048, 512],
    "we_down": [cfg.DEPTH, cfg.NE, 512, 2048], "ln2_g": [cfg.DEPTH, 2048], "ln2_b": [cfg.DEPTH, 2048],
}


def build(cfg, stop_after=None):
    S, D, NT, NQB, NCMP, NSLC, NE, NG, CAP = cfg.S, cfg.D, cfg.NT, cfg.NQB, cfg.NCMP, cfg.NSLC, cfg.NE, cfg.NG, cfg.CAP
    nc = bass.Bass("TRN2", target_bir_lowering=False)
    top = contextlib.ExitStack()
    dbg = {}
    with top:
        fw = FW(nc, top)
        x_in = nc.dram_tensor("x", [S, D], F32, kind="ExternalInput").ap()
        Wd = {k: nc.dram_tensor(k, shp, F32, kind="ExternalInput").ap() for k, shp in WEIGHT_SHAPES(cfg).items()}
        hc = host_consts(cfg)
        Cd = {k: nc.dram_tensor("c_" + k, list(v.shape), CONST_DT[k], kind="ExternalInput").ap() for k, v in hc.items()}
        y_out = nc.dram_tensor("y", [S, D], F32, kind="ExternalOutput").ap()

        def scratch(name, shape, dt):
            if cfg.debug:
                t = nc.dram_tensor(name, shape, dt, kind="ExternalOutput").ap()
                dbg[name] = t
                return t
            return nc.dram_tensor(name, shape, dt).ap()

        projT = scratch("projT", [NCHUNK * 128, S], BF16)
        vtok = scratch("vtok", [S, 384], BF16)
        gates = scratch("gates", [S, 48], F32)
        otok = scratch("otok", [S, D], BF16)
        x1d = scratch("x1d", [S, D], F32)
        xmid = scratch("xmid", [S, D], F32)
        xd = scratch("xd", [NE * CAP, D], BF16)
        yd = scratch("yd", [NE * CAP, D], F32)
        b_projT, b_vtok, b_gates, b_otok, b_x1d, b_xmid, b_xd, b_yd = [Buf(n) for n in
                                                                      ("projT", "vtok", "gates", "otok", "x1d", "xmid", "xd", "yd")]
        if cfg.debug:
            scratch("kcdbg", [2, 69, 256], BF16)
            scratch("vcdbg", [2, 128, 2, 64], BF16)
        b_xin = Buf("xin")
        b_yout = Buf("yout")

        _uid = [0]

        def sb(es, name, shape, dt):
            _uid[0] += 1
            return es.enter_context(nc.sbuf_tensor(f"{name}_{_uid[0]}", shape, dt))

        PS = [top.enter_context(nc.psum_tensor(f"ps{i}", [128, 512], F32)) for i in range(8)]
        bPS = [Buf(f"ps{i}") for i in range(8)]

        bc_reg = nc.gpsimd.to_reg(NE * CAP - 1)
        ident_b = sb(top, "ident_b", [128, 128], BF16)
        ident_f = sb(top, "ident_f", [128, 128], F32)
        bC = Buf("consts")
        fw.dma("sp", lambda e: e.dma_start(out=ident_b[:], in_=Cd["identb"][:, :]), pwrites=[bC])
        fw.dma("sp", lambda e: e.dma_start(out=ident_f[:], in_=Cd["identf"][:, :]), pwrites=[bC])

        def mm(out, lhsT, rhs, start, stop, reads, bank, first):
            if first:
                fw.op("pe", lambda e: e.matmul(out, lhsT, rhs, start=start, stop=stop), reads=reads, writes=[bank])
            else:
                fw.op("pe", lambda e: e.matmul(out, lhsT, rhs, start=start, stop=stop), reads=reads, pwrites=[bank])

        def phase_inproj(l, xsrc, b_xsrc):
            with contextlib.ExitStack() as ph:
                w_sb = sb(ph, "w_in_sb", [128, 16, 3120], BF16)
                b_w = Buf("w_in")
                wsrc = Wd["w_in"][l].rearrange("(k p) c -> p k c", p=128)
                for c0 in (0, 1560):
                    fw.dma("pool", lambda e: e.dma_start(out=w_sb[:, :, c0:c0 + 1560], in_=wsrc[:, :, c0:c0 + 1560]), pwrites=[b_w])
                bias_fm = sb(ph, "bias_fm", [128, NCHUNK], F32)
                b_bias = Buf("bias")
                with nc.allow_non_contiguous_dma(reason="tiny bias column loads"):
                    for ci, c0 in enumerate(SRC_COLS):
                        fw.dma("sp", lambda e: e.dma_start(out=bias_fm[:, ci:ci + 1],
                                                           in_=Wd["b_in"][l, c0:c0 + 128].rearrange("(p o) -> p o", o=1)), pwrites=[b_bias])
                bias_tm = sb(ph, "bias_tm", [128, 432], F32)
                for (d0, s0, n) in ((0, 1152, 128), (128, 2688, 128), (256, 2944, 176)):
                    fw.dma("sp", lambda e: e.dma_start(out=bias_tm[:, d0:d0 + n], in_=Wd["b_in"][l, s0:s0 + n].partition_broadcast(128)), pwrites=[b_bias])
                xt = [sb(ph, f"xt{i}", [128, D], F32) for i in range(2)]
                b_xt = [Buf(f"xt{i}") for i in range(2)]
                xT = sb(ph, "xT", [128, 16, 512], BF16)
                b_xT = Buf("xT")
                stg = [sb(ph, f"stg{i}", [128, 512], BF16) for i in range(2)]
                b_stg = [Buf(f"stg{i}") for i in range(2)]
                vst = [sb(ph, f"vst{i}", [128, 384], BF16) for i in range(2)]
                b_vst = [Buf(f"vst{i}") for i in range(2)]
                gst = [sb(ph, f"gst{i}", [128, 48], F32) for i in range(2)]
                b_gst = [Buf(f"gst{i}") for i in range(2)]
                gtmp = sb(ph, "gtmp", [128, 48], F32)
                b_gtmp = Buf("gtmp")
                nload = 0
                nstg = 0
                nv = 0
                for tb in range(NQB):
                    for j in range(4):
                        ti = tb * 4 + j
                        s = nload % 2
                        nload += 1
                        fw.dma("sp", lambda e: e.dma_start(out=xt[s][:], in_=xsrc[ti * 128:(ti + 1) * 128, :]), reads=[b_xsrc], writes=[b_xt[s]])
                        for kq in range(4):
                            bk = (j * 4 + kq) % 4
                            for kk in range(4):
                                k = kq * 4 + kk
                                fw.op("pe", lambda e: e.transpose(PS[bk][:, kk * 128:(kk + 1) * 128], xt[s][:, k * 128:(k + 1) * 128], ident_f[:]),
                                      reads=[b_xt[s], bC], **({"writes": [bPS[bk]]} if kk == 0 else {"pwrites": [bPS[bk]]}))
                            src = PS[bk][:, :].rearrange("p (k t) -> p k t", k=4)
                            dst = xT[:, kq * 4:(kq + 1) * 4, j * 128:(j + 1) * 128]
                            if kq % 2 == 0:
                                fw.op("act", lambda e: e.activation(out=dst, in_=src, func=AF.Copy), reads=[bPS[bk]], pwrites=[b_xT])
                            else:
                                fw.op("dve", lambda e: e.tensor_copy(out=dst, in_=src), reads=[bPS[bk]], pwrites=[b_xT])
                    for ci, c0 in enumerate(SRC_COLS):
                        bk = 4 + (ci % 2)
                        for k in range(16):
                            mm(PS[bk][:, :], w_sb[:, k, c0:c0 + 128], xT[:, k, :], k == 0, k == 15, [b_w, b_xT], bPS[bk], k == 0)
                        s = nstg % 2
                        nstg += 1
                        fw.op("act", lambda e: e.activation(out=stg[s][:], in_=PS[bk][:, :], func=AF.Identity, bias=bias_fm[:, ci:ci + 1], scale=1.0),
                              reads=[bPS[bk], b_bias], writes=[b_stg[s]])
                        fw.dma("sp", lambda e: e.dma_start(out=projT[ci * 128:(ci + 1) * 128, tb * 512:(tb + 1) * 512], in_=stg[s][:]),
                               reads=[b_stg[s]], pwrites=[b_projT])
                    for j in range(4):
                        ti = tb * 4 + j
                        bk = 6 + (j % 2)
                        first = True
                        for (d0, s0, n) in ((0, 1152, 128), (128, 2688, 128), (256, 2944, 176)):
                            for k in range(16):
                                mm(PS[bk][:, d0:d0 + n], xT[:, k, j * 128:(j + 1) * 128], w_sb[:, k, s0:s0 + n], k == 0, k == 15,
                                   [b_w, b_xT], bPS[bk], first)
                                first = False
                        s = nv % 2
                        nv += 1
                        fw.op("dve", lambda e: e.tensor_tensor(out=vst[s][:], in0=PS[bk][:, 0:384], in1=bias_tm[:, 0:384], op=ALU.add),
                              reads=[bPS[bk], b_bias], writes=[b_vst[s]])
                        fw.op("dve", lambda e: e.tensor_tensor(out=gtmp[:], in0=PS[bk][:, 384:432], in1=bias_tm[:, 384:432], op=ALU.add),
                              reads=[bPS[bk], b_bias], writes=[b_gtmp])
                        fw.op("act", lambda e: e.activation(out=gst[s][:], in_=gtmp[:], func=AF.Sigmoid), reads=[b_gtmp], writes=[b_gst[s]])
                        fw.dma("sp", lambda e: e.dma_start(out=vtok[ti * 128:(ti + 1) * 128, :], in_=vst[s][:]), reads=[b_vst[s]], pwrites=[b_vtok])
                        fw.dma("sp", lambda e: e.dma_start(out=gates[ti * 128:(ti + 1) * 128, :], in_=gst[s][:]), reads=[b_gst[s]], pwrites=[b_gates])
                fw.barrier()

        NTILES_C = [(0, min(128, NCMP))] + ([(128, NCMP - 128)] if NCMP > 128 else [])

        def phase_compress(l, kcT, vc, b_kc):
            with contextlib.ExitStack() as ph:
                w1 = sb(ph, "cw1", [64, 32, 256], BF16)
                w2 = sb(ph, "cw2", [128, 2, 64], BF16)
                peT = sb(ph, "cpeT", [64, 32], F32)
                kvT = sb(ph, "ckvT", [64, S], BF16)
                kvpe = sb(ph, "ckvpe", [64, 32, 256], BF16)
                hT = sb(ph, "chT", [128, 2, 256], BF16)
                xs = sb(ph, "cxs", [128, 256], F32)
                t1 = sb(ph, "ct1", [128, 256], F32)
                sg = sb(ph, "csg", [128, 256], F32)
                b_w, b_kvT, b_kvpe, b_hT, b_xs, b_t1, b_sg = [Buf(n) for n in ("cw", "kvT", "kvpe", "hT", "xs", "t1", "sg")]
                for kv in (0, 1):
                    sfx = "k" if kv == 0 else "v"
                    fw.dma("pool", lambda e: e.dma_start(out=w1[:], in_=Wd["cmp_w1_" + sfx][l].rearrange("(l d) h -> d l h", d=64)), writes=[b_w])
                    fw.dma("pool", lambda e: e.dma_start(out=w2[:], in_=Wd["cmp_w2_" + sfx][l].rearrange("(c p) o -> p c o", p=128)), pwrites=[b_w])
                    with nc.allow_non_contiguous_dma(reason="tiny pe transpose load"):
                        fw.dma("sp", lambda e: e.dma_start(out=peT[:], in_=Wd["cmp_pe_" + sfx][l].rearrange("l d -> d l")), pwrites=[b_w])
                    for g in (0, 1):
                        row0 = (ROW_KC if kv == 0 else ROW_VC) + g * 64
                        fw.dma("sp", lambda e: e.dma_start(out=kvT[:], in_=projT[row0:row0 + 64, :]), reads=[b_projT], writes=[b_kvT])
                        for ll in range(32):
                            fw.op("act", lambda e: e.activation(out=kvpe[:, ll, 0:NCMP], in_=kvT[:, ll:ll + 16 * (NCMP - 1) + 1:16],
                                                                func=AF.Identity, bias=peT[:, ll:ll + 1], scale=1.0),
                                  reads=[b_kvT, b_w], **({"writes": [b_kvpe]} if ll == 0 else {"pwrites": [b_kvpe]}))
                        for hc2 in (0, 1):
                            for ll in range(32):
                                mm(PS[hc2][:, 0:NCMP], w1[:, ll, hc2 * 128:(hc2 + 1) * 128], kvpe[:, ll, 0:NCMP], ll == 0, ll == 31,
                                   [b_w, b_kvpe], bPS[hc2], ll == 0)
                            fw.op("act", lambda e: e.activation(out=xs[:, 0:NCMP], in_=PS[hc2][:, 0:NCMP], func=AF.Copy), reads=[bPS[hc2]], writes=[b_xs])
                            fw.op("dve", lambda e: e.tensor_tensor(out=t1[:, 0:NCMP], in0=xs[:, 0:NCMP], in1=xs[:, 0:NCMP], op=ALU.mult), reads=[b_xs], writes=[b_t1])
                            fw.op("dve", lambda e: e.tensor_scalar(out=t1[:, 0:NCMP], in0=t1[:, 0:NCMP], scalar1=0.044715, scalar2=1.0, op0=ALU.mult, op1=ALU.add),
                                  reads=[b_t1], writes=[b_t1])
                            fw.op("dve", lambda e: e.tensor_tensor(out=t1[:, 0:NCMP], in0=t1[:, 0:NCMP], in1=xs[:, 0:NCMP], op=ALU.mult), reads=[b_t1, b_xs], writes=[b_t1])
                            fw.op("act", lambda e: e.activation(out=sg[:, 0:NCMP], in_=t1[:, 0:NCMP], func=AF.Sigmoid, scale=1.5957691216057308),
                                  reads=[b_t1], writes=[b_sg])
                            fw.op("dve", lambda e: e.tensor_tensor(out=hT[:, hc2, 0:NCMP], in0=xs[:, 0:NCMP], in1=sg[:, 0:NCMP], op=ALU.mult),
                                  reads=[b_xs, b_sg], **({"writes": [b_hT]} if hc2 == 0 else {"pwrites": [b_hT]}))
                        if kv == 0:
                            for hc2 in (0, 1):
                                mm(PS[2][0:64, 0:NCMP], w2[:, hc2, :], hT[:, hc2, 0:NCMP], hc2 == 0, hc2 == 1, [b_w, b_hT], bPS[2], hc2 == 0)
                            fw.op("act", lambda e: e.activation(out=kcT[g][0:64, 0:NCMP], in_=PS[2][0:64, 0:NCMP], func=AF.Copy), reads=[bPS[2]], pwrites=[b_kc])
                        else:
                            for nti, (n0, rows) in enumerate(NTILES_C):
                                for hc2 in (0, 1):
                                    mm(PS[3][0:rows, nti * 64:(nti + 1) * 64], hT[:, hc2, n0:n0 + rows], w2[:, hc2, :], hc2 == 0, hc2 == 1,
                                       [b_w, b_hT], bPS[3], (nti == 0 and hc2 == 0))
                                fw.op("act", lambda e: e.activation(out=vc[g][0:rows, nti, :], in_=PS[3][0:rows, nti * 64:(nti + 1) * 64], func=AF.Copy),
                                      reads=[bPS[3]], pwrites=[b_kc])
                fw.barrier()

        def phase_attention(l):
            with contextlib.ExitStack() as ph:
                kcT = [sb(ph, f"kcT{g}", [69, 256], BF16) for g in (0, 1)]
                vc = [sb(ph, f"vc{g}", [128, 2, 64], BF16) for g in (0, 1)]
                b_kc = Buf("kc")
                for g in (0, 1):
                    fw.dma("sp", lambda e: e.dma_start(out=kcT[g][64:69, :], in_=Cd["kaugc"][:, :]), pwrites=[b_kc])
                phase_compress(l, kcT, vc, b_kc)
                if cfg.debug:
                    for g in (0, 1):
                        fw.dma("sp", lambda e: e.dma_start(out=dbg["kcdbg"][g], in_=kcT[g][:, :]), reads=[b_kc])
                        fw.dma("sp", lambda e: e.dma_start(out=dbg["vcdbg"][g], in_=vc[g][:, :, :]), reads=[b_kc])
                b_K = Buf("KV")
                KT = {}
                VV = {}
                for nm, row0, voff in (("a", ROW_KA, 0), ("s", ROW_KS, 128), ("w", ROW_KW, 256)):
                    for g in (0, 1):
                        kt_ = sb(ph, f"KT{nm}{g}", [69, S], BF16)
                        vv_ = sb(ph, f"VV{nm}{g}", [128, NT, 65], BF16)
                        KT[nm, g] = kt_
                        VV[nm, g] = vv_
                        r0 = row0 + g * 64
                        fw.dma("sp", lambda e: e.dma_start(out=kt_[0:64, :], in_=projT[r0:r0 + 64, :]), reads=[b_projT], pwrites=[b_K])
                        fw.dma("sp", lambda e: e.dma_start(out=kt_[64:69, :], in_=Cd["kaug"][:, :]), pwrites=[b_K])
                        c0 = voff + g * 64
                        with nc.allow_non_contiguous_dma(reason="v token-major tiles (128B rows)"):
                            fw.dma("sp", lambda e: e.dma_start(out=vv_[:, :, 0:64], in_=vtok[:, c0:c0 + 64].rearrange("(t p) c -> p t c", p=128)),
                                   reads=[b_vtok], pwrites=[b_K])
                        fw.op("dve", lambda e: e.memset(vv_[:, :, 64:65], 1.0), pwrites=[b_K])
                caus = sb(ph, "caus", [128, 128], BF16)
                anti = sb(ph, "anti", [128, 128], BF16)
                swam = sb(ph, "swam", [128, 256], BF16)
                WCM = 8 * (NT - 1) + NCMP
                cmpm = sb(ph, "cmpm", [128, WCM], BF16)
                eblk = sb(ph, "eblk", [NSLC, S], BF16)
                selb = sb(ph, "selb", [128, NT, NSLC], F32)
                esink = sb(ph, "esink", [128, 16], F32)
                for t_, nm in ((caus, "caus"), (anti, "anti"), (swam, "swam"), (cmpm, "cmpm"), (eblk, "eblk")):
                    fw.dma("sp", lambda e: e.dma_start(out=t_[:], in_=Cd[nm][:, :]), pwrites=[b_K])
                fw.dma("sp", lambda e: e.dma_start(out=selb[:], in_=Cd["selbias"][:, :, :]), pwrites=[b_K])
                b_es = Buf("esink")
                fw.dma("sp", lambda e: e.dma_start(out=esink[:], in_=Wd["swa_sinks"][l, :].partition_broadcast(128)), writes=[b_es])
                fw.op("act", lambda e: e.activation(out=esink[:], in_=esink[:], func=AF.Exp), reads=[b_es], writes=[b_es])

                QT = sb(ph, "QT", [69, 32, 512], BF16)
                b_QT = Buf("QT")
                gt = sb(ph, "gt", [128, 4, 48], F32)
                b_gt = Buf("gt")
                ofp = sb(ph, "ofp", [128, 4, 16, 64], F32)
                b_ofp = Buf("ofp")
                otile = sb(ph, "otile", [128, 4, 2048], BF16)
                b_ot = Buf("otile")
                PT = [sb(ph, f"PT{i}", [128, 512], BF16) for i in range(3)]
                b_PT = [Buf(f"PT{i}") for i in range(3)]
                nselT = [sb(ph, f"nselT{g}", [NSLC, 512], BF16) for g in (0, 1)]
                b_ns = [Buf(f"nselT{g}") for g in (0, 1)]
                pcs = sb(ph, "pcs", [128, 4 * NSLC + 4], F32)
                b_pcs = Buf("pcs")
                pf = [sb(ph, f"pf{i}", [128, 256], F32) for i in range(2)]
                b_pf = [Buf(f"pf{i}") for i in range(2)]
                pcb = [sb(ph, f"pcb{i}", [128, 256], BF16) for i in range(2)]
                b_pcb = [Buf(f"pcb{i}") for i in range(2)]
                pcT = [sb(ph, f"pcT{i}", [128, 2, 128], BF16) for i in range(2)]
                b_pcT = [Buf(f"pcT{i}") for i in range(2)]
                den = [sb(ph, f"den{i}", [128, 8], F32) for i in range(4)]
                b_den = [Buf(f"den{i}") for i in range(4)]
                imp = sb(ph, "imp", [128, NSLC], F32)
                sc2 = sb(ph, "sc2", [128, NSLC], F32)
                m8 = sb(ph, "m8", [128, 16], F32)
                msk = sb(ph, "msk", [128, 2, NSLC], F32)
                nsel = sb(ph, "nsel", [128, NSLC], BF16)
                b_tk = Buf("topk")
                cnt = {"st": 0, "acc": 0, "pt": 0, "c1": 0, "den": 0}

                def st_tile():
                    i = cnt["st"] % 3
                    cnt["st"] += 1
                    return PS[i], bPS[i]

                def acc_tile():
                    i = 3 + cnt["acc"] % 2
                    cnt["acc"] += 1
                    return PS[i], bPS[i]

                def pt_tile():
                    i = cnt["pt"] % 3
                    cnt["pt"] += 1
                    return PT[i], b_PT[i]

                def den_tile():
                    i = cnt["den"] % 4
                    cnt["den"] += 1
                    return den[i], b_den[i]

                S5 = [PS[5], PS[0]]
                bS5 = [bPS[5], bPS[0]]
                P6 = [PS[6][:, :].bitcast(BF16), PS[1][:, :].bitcast(BF16)]
                bP6 = [bPS[6], bPS[1]]
                P7 = [PS[7], PS[2]]
                bP7 = [bPS[7], bPS[2]]
                P7n = PS[3][:, :].bitcast(BF16)
                bP7n = bPS[3]
                pb = [sb(ph, f"pbb{i}", [128, 256], BF16) for i in range(2)]
                b_pb = [Buf(f"pbb{i}") for i in range(2)]
                den8 = [sb(ph, f"den8_{i}", [128, 8], F32) for i in range(8)]
                b_den8 = [Buf(f"den8_{i}") for i in range(8)]
                LOOK = cfg.look

                class Job:
                    __slots__ = ("s1", "s2")

                def make_job(H, ktile_ap, c0, c1, masks, vtile_ap, acc, bacc, subtiles, pv_state, extra_reads, fin):
                    jb = Job()
                    hold = {}

                    def s1():
                        st, bst = st_tile()
                        nm = len(masks)
                        mm(st[:, c0:c1], ktile_ap, QT[:, H, c0:c1], True, nm == 0, [b_K, b_QT] + extra_reads, bst, True)
                        for mi, (lhsT, rhs, m0, m1) in enumerate(masks):
                            mm(st[:, m0:m1], lhsT, rhs, False, mi == nm - 1, [b_K, bC] + extra_reads, bst, False)
                        pt, bpt = pt_tile()
                        fw.op("act", lambda e: e.activation(out=pt[:, c0:c1], in_=st[:, c0:c1], func=AF.Exp, scale=0.125), reads=[bst], writes=[bpt])
                        hold["pt"] = (pt, bpt)

                    def s2():
                        pt, bpt = hold["pt"]
                        for j in subtiles:
                            first = pv_state["first"]
                            pv_state["first"] = False
                            pv_state["n"] -= 1
                            last = pv_state["n"] == 0
                            mm(acc[:, j * 128:j * 128 + 65], pt[:, j * 128:(j + 1) * 128], vtile_ap, first, last, [bpt, b_K], bacc, first)
                        if fin is not None and pv_state["n"] == 0:
                            fin()

                    jb.s1 = s1
                    jb.s2 = s2
                    return jb

                def run_jobs(jobs):
                    n = len(jobs)
                    for i in range(n + LOOK):
                        if i < n:
                            jobs[i].s1()
                        if i - LOOK >= 0:
                            jobs[i - LOOK].s2()

                for qb in range(NQB):
                    q0 = qb * 512
                    fw.dma("sp", lambda e: e.dma_start(out=QT[0:64, :, :], in_=projT[0:2048, q0:q0 + 512].rearrange("(h d) s -> d h s", d=64)),
                           reads=[b_projT], writes=[b_QT])
                    fw.dma("sp", lambda e: e.dma_start(out=QT[64:69, :, :], in_=Cd["qaug"][:, :, q0:q0 + 512]), pwrites=[b_QT])
                    fw.dma("sp", lambda e: e.dma_start(out=gt[:], in_=gates[q0:q0 + 512, :].rearrange("(j p) c -> p j c", p=128)), reads=[b_gates], writes=[b_gt])
                    ofp_state = {"first": True, "init": set()}

                    def ofp_write(j, hn, in0_ap, scal_ap, reads):
                        if (j, hn) in ofp_state["init"]:
                            fw.op("dve", lambda e: e.scalar_tensor_tensor(out=ofp[:, j, hn, :], in0=in0_ap, scalar=scal_ap, in1=ofp[:, j, hn, :],
                                                                          op0=ALU.mult, op1=ALU.add), reads=reads, pwrites=[b_ofp])
                        else:
                            fw.op("dve", lambda e: e.tensor_scalar(out=ofp[:, j, hn, :], in0=in0_ap, scalar1=scal_ap, scalar2=None, op0=ALU.mult),
                                  reads=reads, **({"writes": [b_ofp]} if ofp_state["first"] else {"pwrites": [b_ofp]}))
                            ofp_state["first"] = False
                            ofp_state["init"].add((j, hn))

                    p6 = PS[6][:, :].bitcast(BF16)
                    p7 = PS[7][:, :].bitcast(BF16)
                    for g in (0, 1):
                        for j in range(4):
                            qt = qb * 4 + j
                            moff = 8 * (NT - 1 - qt)
                            fw.op("dve", lambda e: e.memset(pcs[:], 0.0), writes=[b_pcs])
                            dnr = {}

                            def stA(r):
                                hn = g * 8 + r
                                H = 16 + hn
                                a = r % 2
                                reg = S5[a][:, 0:NCMP]
                                mm(reg, QT[:, H, j * 128:(j + 1) * 128], kcT[g][:, 0:NCMP], True, False, [b_QT, b_kc], bS5[a], True)
                                mm(reg, ident_b[:], cmpm[:, moff:moff + NCMP], False, True, [bC, b_K], bS5[a], False)
                                i8 = cnt["den"] % 8
                                cnt["den"] += 1
                                dn, bdn = den8[i8], b_den8[i8]
                                dnr[r] = (dn, bdn)
                                fw.op("act", lambda e: e.activation(out=pb[a][:, 0:NCMP], in_=reg, func=AF.Exp, scale=0.125),
                                      reads=[bS5[a]], writes=[b_pb[a]])
                                fw.op("dve", lambda e: e.tensor_reduce(out=dn[:, 0:1], in_=pb[a][:, 0:NCMP], axis=AX.X, op=ALU.add), reads=[b_pb[a]], writes=[bdn])
                                fw.op("dve", lambda e: e.tensor_scalar(out=dn[:, 1:2], in0=dn[:, 0:1], scalar1=1e-30, scalar2=None, op0=ALU.max), reads=[bdn], writes=[bdn])
                                fw.op("dve", lambda e: e.reciprocal(out=dn[:, 2:3], in_=dn[:, 1:2]), reads=[bdn], writes=[bdn])
                                fw.op("dve", lambda e: e.tensor_tensor(out=dn[:, 3:4], in0=dn[:, 2:3], in1=gt[:, j, hn * 3:hn * 3 + 1], op=ALU.mult), reads=[bdn, b_gt], writes=[bdn])
                                fw.op("dve", lambda e: e.scalar_tensor_tensor(out=pcs[:, 1:1 + NCMP], in0=pb[a][:, 0:NCMP], scalar=dn[:, 2:3], in1=pcs[:, 1:1 + NCMP],
                                                                              op0=ALU.mult, op1=ALU.add), reads=[b_pb[a], bdn], writes=[b_pcs])

                            def stC(r):
                                a = r % 2
                                for nti, (n0, rows) in enumerate(NTILES_C):
                                    fw.op("pe", lambda e: e.transpose(P6[a][0:rows, nti * 128:(nti + 1) * 128], pb[a][:, n0:n0 + rows], ident_b[:]),
                                          reads=[b_pb[a], bC], **({"writes": [bP6[a]]} if nti == 0 else {"pwrites": [bP6[a]]}))
                                for nti, (n0, rows) in enumerate(NTILES_C):
                                    fw.op("act", lambda e: e.activation(out=pcT[a][0:rows, nti, :], in_=P6[a][0:rows, nti * 128:(nti + 1) * 128], func=AF.Copy),
                                          reads=[bP6[a]], **({"writes": [b_pcT[a]]} if nti == 0 else {"pwrites": [b_pcT[a]]}))

                            def stD(r):
                                hn = g * 8 + r
                                a = r % 2
                                dn, bdn = dnr[r]
                                reg = P7[a][:, 0:64]
                                for nti, (n0, rows) in enumerate(NTILES_C):
                                    mm(reg, pcT[a][0:rows, nti, :], vc[g][0:rows, nti, :], nti == 0, nti == len(NTILES_C) - 1,
                                       [b_pcT[a], b_kc], bP7[a], nti == 0)
                                if 0 in cfg.branches:
                                    ofp_write(j, hn, reg, dn[:, 3:4], [bP7[a], bdn])

                            if cfg.c1skew:
                                for step in range(8 + 2):
                                    if step < 8:
                                        stA(step)
                                    if 0 <= step - 1 < 8:
                                        stC(step - 1)
                                    if 0 <= step - 2 < 8:
                                        stD(step - 2)
                            else:
                                for step in range(8):
                                    stA(step)
                                    stC(step)
                                    stD(step)
                            fw.op("dve", lambda e: e.tensor_reduce(out=imp[:], in_=pcs[:, 0:4 * NSLC].rearrange("p (j i) -> p j i", i=4), axis=AX.X, op=ALU.add),
                                  reads=[b_pcs], writes=[b_tk])
                            fw.op("dve", lambda e: e.tensor_tensor(out=imp[:], in0=imp[:], in1=pcs[:, 4:4 * NSLC + 1:4], op=ALU.add), reads=[b_pcs, b_tk], writes=[b_tk])
                            fw.op("dve", lambda e: e.tensor_tensor(out=imp[:], in0=imp[:], in1=selb[:, qt, :], op=ALU.add), reads=[b_K, b_tk], writes=[b_tk])
                            fw.op("dve", lambda e: e.max(out=m8[:, 0:8], in_=imp[:]), reads=[b_tk], writes=[b_tk])
                            if cfg.KSEL == 16:
                                fw.op("dve", lambda e: e.match_replace(out=sc2[:], in_to_replace=m8[:, 0:8], in_values=imp[:], imm_value=-3e9), reads=[b_tk], writes=[b_tk])
                                fw.op("dve", lambda e: e.max(out=m8[:, 8:16], in_=sc2[:]), reads=[b_tk], writes=[b_tk])
                                thr = m8[:, 15:16]
                            else:
                                thr = m8[:, 7:8]
                            fw.op("dve", lambda e: e.tensor_scalar(out=msk[:, 0, :], in0=imp[:], scalar1=thr, scalar2=None, op0=ALU.is_ge), reads=[b_tk], writes=[b_tk])
                            fw.op("dve", lambda e: e.tensor_scalar(out=msk[:, 1, :], in0=imp[:], scalar1=-5e8, scalar2=None, op0=ALU.is_gt), reads=[b_tk], writes=[b_tk])
                            fw.op("dve", lambda e: e.tensor_tensor(out=msk[:, 0, :], in0=msk[:, 0, :], in1=msk[:, 1, :], op=ALU.mult), reads=[b_tk], writes=[b_tk])
                            fw.op("dve", lambda e: e.tensor_scalar(out=nsel[:], in0=msk[:, 0, :], scalar1=1.0, scalar2=-NEG, op0=ALU.subtract, op1=ALU.mult),
                                  reads=[b_tk], writes=[b_tk])
                            fw.op("pe", lambda e: e.transpose(P7n[0:NSLC, 0:128], nsel[:], ident_b[:]), reads=[b_tk, bC], writes=[bP7n])
                            fw.op("act", lambda e: e.activation(out=nselT[g][:, j * 128:(j + 1) * 128], in_=P7n[0:NSLC, 0:128], func=AF.Copy),
                                  reads=[bP7n], **({"writes": [b_ns[g]]} if j == 0 else {"pwrites": [b_ns[g]]}))
                    jobs = []
                    ot_state = {"first": True}
                    for g in (0, 1):
                        for r in range(8):
                            hn = g * 8 + r
                            H = 16 + hn
                            for br in (2, 1):
                                if br not in cfg.branches:
                                    continue
                                acc, bacc = acc_tile()
                                tiles = []
                                if br == 2:
                                    for i in range(4):
                                        kt = qb * 4 - 4 + i
                                        if kt >= 0:
                                            tiles.append((kt, 0, 128 * (i + 1), [(ident_b[:], anti[:], 128 * i, 128 * (i + 1))], list(range(0, i + 1))))
                                    for i in range(4):
                                        kt = qb * 4 + i
                                        tiles.append((kt, 128 * i, 512, [(ident_b[:], caus[:], 128 * i, 128 * (i + 1))], list(range(i, 4))))
                                    ktn, vvn = KT["w", g], VV["w", g]
                                else:
                                    for kt in range(qb * 4 + 4):
                                        i = kt - qb * 4
                                        c0 = 0 if i < 0 else 128 * i
                                        ms = [(eblk[:, kt * 128:(kt + 1) * 128], nselT[g][:, c0:512], c0, 512)]
                                        if i >= 0:
                                            ms.append((ident_b[:], caus[:], c0, c0 + 128))
                                        tiles.append((kt, c0, 512, ms, list(range(max(i, 0), 4))))
                                    ktn, vvn = KT["s", g], VV["s", g]
                                pv_state = {"first": True, "n": sum(len(t[4]) for t in tiles)}

                                def fin_nsa(acc=acc, bacc=bacc, hn=hn, br=br):
                                    dn, bdn = den_tile()
                                    accv = acc[:, :].rearrange("p (j c) -> p j c", j=4)
                                    fw.op("dve", lambda e: e.tensor_scalar(out=dn[:, 0:4], in0=accv[:, :, 64], scalar1=1e-30, scalar2=None, op0=ALU.max), reads=[bacc], writes=[bdn])
                                    fw.op("dve", lambda e: e.reciprocal(out=dn[:, 0:4], in_=dn[:, 0:4]), reads=[bdn], writes=[bdn])
                                    fw.op("dve", lambda e: e.tensor_tensor(out=dn[:, 4:8], in0=dn[:, 0:4], in1=gt[:, :, hn * 3 + br], op=ALU.mult), reads=[bdn, b_gt], writes=[bdn])
                                    for j in range(4):
                                        ofp_write(j, hn, accv[:, j, 0:64], dn[:, 4 + j:5 + j], [bacc, bdn])

                                for ti_, (kt, c0, c1, ms, subs) in enumerate(tiles):
                                    jobs.append(make_job(H, ktn[:, kt * 128:(kt + 1) * 128], c0, c1, ms, vvn[:, kt, :], acc, bacc, subs, pv_state,
                                                         [b_ns[g]] if br == 1 else [], fin_nsa))
                    for g in (0, 1):
                        for r in range(8):
                            H = g * 8 + r
                            acc, bacc = acc_tile()
                            tiles = []
                            kt = qb * 4 - 1
                            if kt >= 0:
                                tiles.append((kt, 0, 128, [(ident_b[:], anti[:], 0, 128)], [0]))
                            for i in range(4):
                                kt = qb * 4 + i
                                c1 = min(512, 128 * (i + 2))
                                tiles.append((kt, 128 * i, c1, [(ident_b[:], swam[:, 0:c1 - 128 * i], 128 * i, c1)], list(range(i, min(i + 2, 4)))))
                            pv_state = {"first": True, "n": sum(len(t[4]) for t in tiles)}

                            def fin_swa(acc=acc, bacc=bacc, H=H):
                                dn, bdn = den_tile()
                                accv = acc[:, :].rearrange("p (j c) -> p j c", j=4)
                                fw.op("dve", lambda e: e.tensor_scalar(out=dn[:, 0:4], in0=accv[:, :, 64], scalar1=esink[:, H:H + 1], scalar2=None, op0=ALU.add),
                                      reads=[bacc, b_es], writes=[bdn])
                                fw.op("dve", lambda e: e.reciprocal(out=dn[:, 0:4], in_=dn[:, 0:4]), reads=[bdn], writes=[bdn])
                                for j in range(4):
                                    fw.op("dve", lambda e: e.tensor_scalar(out=otile[:, j, H * 64:(H + 1) * 64], in0=accv[:, j, 0:64], scalar1=dn[:, j:j + 1], scalar2=None, op0=ALU.mult),
                                          reads=[bacc, bdn], **({"writes": [b_ot]} if ot_state["first"] else {"pwrites": [b_ot]}))
                                    ot_state["first"] = False

                            for (kt, c0, c1, ms, subs) in tiles:
                                jobs.append(make_job(H, KT["a", g][:, kt * 128:(kt + 1) * 128], c0, c1, ms, VV["a", g][:, kt, :], acc, bacc, subs, pv_state, [], fin_swa))
                    run_jobs(jobs)
                    fw.op("dve", lambda e: e.tensor_copy(out=otile[:, :, 1024:2048], in_=ofp[:].rearrange("p j h d -> p j (h d)")), reads=[b_ofp], pwrites=[b_ot])
                    fw.dma("sp", lambda e: e.dma_start(out=otok[q0:q0 + 512, :].rearrange("(j p) c -> p j c", p=128), in_=otile[:]), reads=[b_ot], pwrites=[b_otok])
                fw.barrier()

        def layer_norm_tile(yt, b_yt, gb, bb, b_gb, stats, mv, b_st):
            for c4 in range(4):
                fw.op("dve", lambda e: e.bn_stats(out=stats[:, c4, :], in_=yt[:, c4 * 512:(c4 + 1) * 512]), reads=[b_yt],
                      **({"writes": [b_st]} if c4 == 0 else {"pwrites": [b_st]}))
            fw.op("dve", lambda e: e.bn_aggr(out=mv[:, 0:2], in_=stats[:].rearrange("p a b -> p (a b)")), reads=[b_st], writes=[b_st])
            fw.op("dve", lambda e: e.tensor_scalar(out=mv[:, 2:3], in0=mv[:, 1:2], scalar1=1e-5, scalar2=None, op0=ALU.add), reads=[b_st], writes=[b_st])
            fw.op("act", lambda e: e.activation(out=mv[:, 2:3], in_=mv[:, 2:3], func=AF.Sqrt), reads=[b_st], writes=[b_st])
            fw.op("dve", lambda e: e.reciprocal(out=mv[:, 3:4], in_=mv[:, 2:3]), reads=[b_st], writes=[b_st])
            fw.op("dve", lambda e: e.tensor_scalar(out=yt[:], in0=yt[:], scalar1=mv[:, 0:1], scalar2=mv[:, 3:4], op0=ALU.subtract, op1=ALU.mult),
                  reads=[b_st, b_yt], writes=[b_yt])
            fw.op("pool", lambda e: e.tensor_tensor(out=yt[:], in0=yt[:], in1=gb[:], op=ALU.mult), reads=[b_yt, b_gb], writes=[b_yt])
            fw.op("pool", lambda e: e.tensor_tensor(out=yt[:], in0=yt[:], in1=bb[:], op=ALU.add), reads=[b_yt, b_gb], writes=[b_yt])

        NR = NG + NE

        def phase_outproj_router(l, xsrc, b_xsrc, destI, wts, b_rt):
            with contextlib.ExitStack() as ph:
                wo = sb(ph, "wo", [128, 16, 2048], BF16)
                b_wo = Buf("wo")
                fw.dma("pool", lambda e: e.dma_start(out=wo[:], in_=Wd["w_out"][l].rearrange("(k p) c -> p k c", p=128)), writes=[b_wo])
                bo = sb(ph, "bo", [128, D], F32)
                g1 = sb(ph, "g1", [128, D], F32)
                b1 = sb(ph, "b1", [128, D], F32)
                b_gb = Buf("gb")
                for t_, nm in ((bo, "b_out"), (g1, "ln1_g"), (b1, "ln1_b")):
                    fw.dma("sp", lambda e: e.dma_start(out=t_[:], in_=Wd[nm][l, :].partition_broadcast(128)), pwrites=[b_gb])
                wr = sb(ph, "wr", [128, 16, NR], F32)
                rb = sb(ph, "rb", [128, NR], F32)
                with nc.allow_non_contiguous_dma(reason="small router weight rows"):
                    fw.dma("sp", lambda e: e.dma_start(out=wr[:, :, 0:NG], in_=Wd["w_group"][l].rearrange("(k p) c -> p k c", p=128)), pwrites=[b_gb])
                    fw.dma("sp", lambda e: e.dma_start(out=wr[:, :, NG:NR], in_=Wd["w_expert"][l].rearrange("(k p) c -> p k c", p=128)), pwrites=[b_gb])
                fw.dma("sp", lambda e: e.dma_start(out=rb[:, 0:NG], in_=Wd["b_group"][l, :].partition_broadcast(128)), pwrites=[b_gb])
                fw.dma("sp", lambda e: e.dma_start(out=rb[:, NG:NR], in_=Wd["b_expert"][l, :].partition_broadcast(128)), pwrites=[b_gb])
                ltri = sb(ph, "ltri", [128, 128], F32)
                onesf = sb(ph, "onesf", [128, 128], F32)
                iotae = sb(ph, "iotae", [128, NE], F32)
                fw.dma("sp", lambda e: e.dma_start(out=ltri[:], in_=Cd["ltri"][:, :]), pwrites=[b_gb])
                fw.dma("sp", lambda e: e.dma_start(out=onesf[:], in_=Cd["onesf"][:, :]), pwrites=[b_gb])
                fw.dma("sp", lambda e: e.dma_start(out=iotae[:], in_=Cd["iotae"][:, :]), pwrites=[b_gb])
                ot = [sb(ph, f"ot{i}", [128, D], BF16) for i in range(2)]
                xr = [sb(ph, f"xr{i}", [128, D], F32) for i in range(2)]
                b_ot = [Buf(f"ot{i}") for i in range(2)]
                b_xr = [Buf(f"xr{i}") for i in range(2)]
                oT = sb(ph, "oT", [128, 16, 128], BF16)
                b_oT = Buf("oT")
                yt = [sb(ph, f"yt{i}", [128, D], F32) for i in range(2)]
                b_yt = [Buf(f"yt{i}") for i in range(2)]
                x1b = [sb(ph, f"x1b{i}", [128, D], BF16) for i in range(2)]
                b_x1b = [Buf(f"x1b{i}") for i in range(2)]
                xT1 = sb(ph, "xT1", [128, 16, 128], F32)
                b_xT1 = Buf("xT1")
                stats = sb(ph, "stats", [128, 4, 6], F32)
                mv = sb(ph, "mv", [128, 4], F32)
                b_st = Buf("st")
                lg = sb(ph, "lg", [128, NR], F32)
                sm = sb(ph, "sm", [128, 16], F32)
                ohg = sb(ph, "ohg", [128, NG], F32)
                ge = sb(ph, "ge", [128, NG], F32)
                esel = sb(ph, "esel", [128, 8], F32)
                m8r = sb(ph, "m8r", [128, 8], F32)
                oh = sb(ph, "oh", [128, 2, 8], F32)
                Ak = sb(ph, "Ak", [128, 2, NE], F32)
                Asum = sb(ph, "Asum", [128, NE], F32)
                Acum = sb(ph, "Acum", [128, NE], F32)
                cntt = sb(ph, "cntt", [128, NE], F32)
                tmpe = sb(ph, "tmpe", [128, NE], F32)
                dstf = sb(ph, "dstf", [128, 2], F32)
                tmp2 = sb(ph, "tmp2", [128, 2, NE], F32)
                sm2 = sb(ph, "sm2", [128, 16], F32)
                b_r = Buf("router")
                b_ac = Buf("acum")
                fw.op("dve", lambda e: e.memset(Acum[:], 0.0), writes=[b_ac])
                def stX(ti):
                    s = ti % 2
                    r0 = ti * 128
                    fw.dma("sp", lambda e: e.dma_start(out=ot[s][:], in_=otok[r0:r0 + 128, :]), reads=[b_otok], writes=[b_ot[s]])
                    fw.dma("sp", lambda e: e.dma_start(out=xr[s][:], in_=xsrc[r0:r0 + 128, :]), reads=[b_xsrc], writes=[b_xr[s]])
                    for kq in range(4):
                        bk = kq % 2
                        pb = PS[bk][:, :].bitcast(BF16)
                        for kk in range(4):
                            k = kq * 4 + kk
                            fw.op("pe", lambda e: e.transpose(pb[:, kk * 128:(kk + 1) * 128], ot[s][:, k * 128:(k + 1) * 128], ident_b[:]),
                                  reads=[b_ot[s], bC], **({"writes": [bPS[bk]]} if kk == 0 else {"pwrites": [bPS[bk]]}))
                        fw.op("act", lambda e: e.activation(out=oT[:, kq * 4:(kq + 1) * 4, :], in_=pb[:, 0:512].rearrange("p (k t) -> p k t", k=4), func=AF.Copy),
                              reads=[bPS[bk]], **({"writes": [b_oT]} if kq == 0 else {"pwrites": [b_oT]}))
                    fw.op("pool", lambda e: e.tensor_scalar(out=xr[s][:], in0=xr[s][:], scalar1=float(cfg.ALPHA), scalar2=None, op0=ALU.mult), reads=[b_xr[s]], writes=[b_xr[s]])
                    fw.op("pool", lambda e: e.tensor_tensor(out=xr[s][:], in0=xr[s][:], in1=bo[:], op=ALU.add), reads=[b_xr[s], b_gb], writes=[b_xr[s]])
                    for cb in range(4):
                        for k in range(16):
                            mm(PS[4 + cb][:, :], oT[:, k, :], wo[:, k, cb * 512:(cb + 1) * 512], k == 0, k == 15, [b_oT, b_wo], bPS[4 + cb], k == 0)

                def stX2(ti):
                    s = ti % 2
                    r0 = ti * 128
                    for cb in range(4):
                        fw.op("dve", lambda e: e.tensor_tensor(out=yt[s][:, cb * 512:(cb + 1) * 512], in0=PS[4 + cb][:, :], in1=xr[s][:, cb * 512:(cb + 1) * 512], op=ALU.add),
                              reads=[bPS[4 + cb], b_xr[s]], **({"writes": [b_yt[s]]} if cb == 0 else {"pwrites": [b_yt[s]]}))
                    layer_norm_tile(yt[s], b_yt[s], g1, b1, b_gb, stats, mv, b_st)
                    fw.dma("sp", lambda e: e.dma_start(out=x1d[r0:r0 + 128, :], in_=yt[s][:]), reads=[b_yt[s]], pwrites=[b_x1d])
                    fw.op("act", lambda e: e.activation(out=x1b[s][:], in_=yt[s][:], func=AF.Copy), reads=[b_yt[s]], writes=[b_x1b[s]])
                def stY(ti):
                    s = ti % 2
                    for kq in range(4):
                        bk = 2 + kq % 2
                        for kk in range(4):
                            k = kq * 4 + kk
                            fw.op("pe", lambda e: e.transpose(PS[bk][:, kk * 128:(kk + 1) * 128], yt[s][:, k * 128:(k + 1) * 128], ident_f[:]),
                                  reads=[b_yt[s], bC], **({"writes": [bPS[bk]]} if kk == 0 else {"pwrites": [bPS[bk]]}))
                        fw.op("act", lambda e: e.activation(out=xT1[:, kq * 4:(kq + 1) * 4, :], in_=PS[bk][:, :].rearrange("p (k t) -> p k t", k=4), func=AF.Copy),
                              reads=[bPS[bk]], **({"writes": [b_xT1]} if kq == 0 else {"pwrites": [b_xT1]}))
                    for k in range(16):
                        mm(PS[2][:, 0:NR], xT1[:, k, :], wr[:, k, :], k == 0, k == 15, [b_xT1, b_gb], bPS[2], k == 0)
                    R = [b_r]
                    fw.op("dve", lambda e: e.tensor_tensor(out=lg[:], in0=PS[2][:, 0:NR], in1=rb[:], op=ALU.add), reads=[bPS[2], b_gb], writes=R)
                    fw.op("dve", lambda e: e.tensor_reduce(out=sm[:, 0:1], in_=lg[:, 0:NG], axis=AX.X, op=ALU.max), reads=R, writes=R)
                    fw.op("dve", lambda e: e.tensor_scalar(out=ohg[:], in0=lg[:, 0:NG], scalar1=sm[:, 0:1], scalar2=None, op0=ALU.is_equal), reads=R, writes=R)
                    fw.op("dve", lambda e: e.tensor_scalar(out=sm[:, 1:2], in0=sm[:, 0:1], scalar1=-1.0, scalar2=None, op0=ALU.mult), reads=R, writes=R)
                    fw.op("act", lambda e: e.activation(out=ge[:], in_=lg[:, 0:NG], func=AF.Exp, bias=sm[:, 1:2], scale=1.0), reads=R, writes=R)
                    fw.op("dve", lambda e: e.tensor_reduce(out=sm[:, 2:3], in_=ge[:], axis=AX.X, op=ALU.add), reads=R, writes=R)
                    fw.op("dve", lambda e: e.reciprocal(out=sm[:, 3:4], in_=sm[:, 2:3]), reads=R, writes=R)
                    lge = lg[:, NG:NR].rearrange("p (g e) -> p g e", e=8)
                    fw.op("dve", lambda e: e.tensor_tensor(out=tmpe[:].rearrange("p (g e) -> p g e", e=8), in0=lge, in1=ohg[:, :].unsqueeze(2).to_broadcast([128, NG, 8]), op=ALU.mult),
                          reads=R, writes=R)
                    fw.op("dve", lambda e: e.tensor_reduce(out=esel[:], in_=tmpe[:].rearrange("p (g e) -> p e g", e=8), axis=AX.X, op=ALU.add), reads=R, writes=R)
                    fw.op("dve", lambda e: e.max(out=m8r[:], in_=esel[:]), reads=R, writes=R)
                    fw.op("dve", lambda e: e.tensor_tensor(out=oh[:], in0=esel[:, :].unsqueeze(1).to_broadcast([128, 2, 8]), in1=m8r[:, 0:2].unsqueeze(2).to_broadcast([128, 2, 8]), op=ALU.is_equal),
                          reads=R, writes=R)
                    fw.op("dve", lambda e: e.tensor_tensor(out=sm[:, 4:5], in0=m8r[:, 1:2], in1=m8r[:, 0:1], op=ALU.subtract), reads=R, writes=R)
                    fw.op("act", lambda e: e.activation(out=sm[:, 5:6], in_=sm[:, 4:5], func=AF.Exp), reads=R, writes=R)
                    fw.op("dve", lambda e: e.tensor_scalar(out=sm[:, 6:7], in0=sm[:, 5:6], scalar1=1.0, scalar2=None, op0=ALU.add), reads=R, writes=R)
                    fw.op("dve", lambda e: e.reciprocal(out=sm[:, 6:7], in_=sm[:, 6:7]), reads=R, writes=R)
                    fw.op("dve", lambda e: e.tensor_tensor(out=sm[:, 7:8], in0=sm[:, 6:7], in1=sm[:, 3:4], op=ALU.mult), reads=R, writes=R)
                    fw.op("dve", lambda e: e.tensor_tensor(out=sm[:, 8:9], in0=sm[:, 7:8], in1=sm[:, 5:6], op=ALU.mult), reads=R, writes=R)
                    for k2 in range(2):
                        fw.op("dve", lambda e: e.tensor_tensor(out=Ak[:, k2, :].rearrange("p (g e) -> p g e", e=8), in0=oh[:, k2, :].unsqueeze(1).to_broadcast([128, NG, 8]),
                                                               in1=ohg[:, :].unsqueeze(2).to_broadcast([128, NG, 8]), op=ALU.mult), reads=R, writes=R)
                    fw.op("dve", lambda e: e.tensor_tensor(out=Asum[:], in0=Ak[:, 0, :], in1=Ak[:, 1, :], op=ALU.add), reads=R, writes=R)
                    mm(PS[3][:, 0:NE], ltri[:], Asum[:], True, False, [b_r, b_gb], bPS[3], True)
                    mm(PS[3][:, 0:NE], onesf[:], Acum[:], False, True, [b_ac, b_gb], bPS[3], False)
                    fw.op("dve", lambda e: e.tensor_copy(out=cntt[:], in_=PS[3][:, 0:NE]), reads=[bPS[3]], writes=R)
                    fw.op("dve", lambda e: e.tensor_tensor(out=Acum[:], in0=Acum[:], in1=Asum[:], op=ALU.add), reads=R + [b_ac], writes=[b_ac])
                    fw.op("dve", lambda e: e.tensor_tensor(out=tmp2[:], in0=Ak[:], in1=cntt[:, :].unsqueeze(1).to_broadcast([128, 2, NE]), op=ALU.mult), reads=R, writes=R)
                    fw.op("dve", lambda e: e.tensor_reduce(out=sm2[:, 0:2], in_=tmp2[:], axis=AX.X, op=ALU.add), reads=R, writes=R)
                    fw.op("dve", lambda e: e.tensor_tensor(out=tmp2[:], in0=Ak[:], in1=iotae[:, :].unsqueeze(1).to_broadcast([128, 2, NE]), op=ALU.mult), reads=R + [b_gb], writes=R)
                    fw.op("dve", lambda e: e.tensor_reduce(out=sm2[:, 2:4], in_=tmp2[:], axis=AX.X, op=ALU.add), reads=R, writes=R)
                    fw.op("dve", lambda e: e.tensor_scalar(out=sm2[:, 4:6], in0=sm2[:, 0:2], scalar1=float(CAP), scalar2=None, op0=ALU.is_lt), reads=R, writes=R)
                    fw.op("dve", lambda e: e.scalar_tensor_tensor(out=sm2[:, 6:8], in0=sm2[:, 2:4], scalar=float(CAP), in1=sm2[:, 0:2], op0=ALU.mult, op1=ALU.add), reads=R, writes=R)
                    fw.op("dve", lambda e: e.tensor_scalar(out=sm2[:, 8:10], in0=sm2[:, 4:6], scalar1=-1e6, scalar2=1e6, op0=ALU.mult, op1=ALU.add), reads=R, writes=R)
                    fw.op("dve", lambda e: e.tensor_tensor(out=dstf[:], in0=sm2[:, 6:8], in1=sm2[:, 8:10], op=ALU.add), reads=R, writes=R)
                    fw.op("dve", lambda e: e.tensor_tensor(out=wts[:, ti, :], in0=sm[:, 7:9], in1=sm2[:, 4:6], op=ALU.mult), reads=R, pwrites=[b_rt])
                    fw.op("dve", lambda e: e.tensor_copy(out=destI[:, ti, :], in_=dstf[:]), reads=R, pwrites=[b_rt])
                    for k2 in range(2):
                        fw.dma("pool", lambda e: e.indirect_dma_start(out=xd[:, :], out_offset=bass.IndirectOffsetOnAxis(ap=destI[:, ti, k2:k2 + 1], axis=0),
                                                                      in_=x1b[s][:], in_offset=None, bounds_check=bc_reg, oob_is_err=False),
                               reads=[b_x1b[s], b_rt], pwrites=[b_xd])
                stX(0)
                stX2(0)
                for ti in range(NT):
                    if ti + 1 < NT:
                        stX(ti + 1)
                    stY(ti)
                    if ti + 1 < NT:
                        stX2(ti + 1)
                fw.barrier()

        CT = CAP // 128

        def phase_experts(l):
            with contextlib.ExitStack() as ph:
                wg = [sb(ph, f"wg{i}", [128, 16, 512], BF16) for i in range(2)]
                wu = [sb(ph, f"wu{i}", [128, 16, 512], BF16) for i in range(2)]
                wdn = [sb(ph, f"wdn{i}", [128, 4, 2048], BF16) for i in range(2)]
                b_wgu = [Buf(f"wgu{i}") for i in range(2)]
                b_wd = [Buf(f"wd{i}") for i in range(2)]
                xe = [sb(ph, f"xe{i}", [128, CT, D], BF16) for i in range(2)]
                b_xe = [Buf(f"xe{i}") for i in range(2)]
                xeT = [sb(ph, f"xeT{i}", [128, 16, CAP], BF16) for i in range(2)]
                b_xeT = [Buf(f"xeT{i}") for i in range(2)]
                hT = [sb(ph, f"ehT{i}", [128, 4, CAP], BF16) for i in range(2)]
                b_hT = [Buf(f"ehT{i}") for i in range(2)]
                sgl = [sb(ph, f"sgl{i}", [128, 512], F32) for i in range(2)]
                b_sgl = [Buf(f"sgl{i}") for i in range(2)]
                hb = [sb(ph, f"hb{i}", [128, 512], BF16) for i in range(2)]
                b_hb = [Buf(f"hb{i}") for i in range(2)]
                ye = [sb(ph, f"ye{i}", [128, D], F32) for i in range(2)]
                b_ye = [Buf(f"ye{i}") for i in range(2)]
                cnt2 = {"sg": 0, "ye": 0}

                def load_gu(e_):
                    if e_ >= NE:
                        return
                    s = e_ % 2
                    fw.dma("pool", lambda e: e.dma_start(out=wg[s][:], in_=Wd["we_gate"][l, e_].rearrange("(k p) c -> p k c", p=128)), writes=[b_wgu[s]])
                    fw.dma("pool", lambda e: e.dma_start(out=wu[s][:], in_=Wd["we_up"][l, e_].rearrange("(k p) c -> p k c", p=128)), pwrites=[b_wgu[s]])

                def load_d(e_):
                    if e_ >= NE:
                        return
                    s = e_ % 2
                    fw.dma("pool", lambda e: e.dma_start(out=wdn[s][:], in_=Wd["we_down"][l, e_].rearrange("(k p) c -> p k c", p=128)), writes=[b_wd[s]])

                def load_x(e_):
                    if e_ >= NE:
                        return
                    s = e_ % 2
                    fw.dma("sp", lambda e: e.dma_start(out=xe[s][:], in_=xd[e_ * CAP:(e_ + 1) * CAP, :].rearrange("(c p) d -> p c d", p=128)), reads=[b_xd], writes=[b_xe[s]])

                def stP(e_):
                    s = e_ % 2
                    for c in range(CT):
                        for kq in range(4):
                            bk = kq % 2
                            pbk = PS[bk][:, :].bitcast(BF16)
                            for kk in range(4):
                                k = kq * 4 + kk
                                fw.op("pe", lambda e: e.transpose(pbk[:, kk * 128:(kk + 1) * 128], xe[s][:, c, k * 128:(k + 1) * 128], ident_b[:]),
                                      reads=[b_xe[s], bC], **({"writes": [bPS[bk]]} if kk == 0 else {"pwrites": [bPS[bk]]}))
                            first = (c == 0 and kq == 0)
                            dst = xeT[s][:, kq * 4:(kq + 1) * 4, c * 128:(c + 1) * 128]
                            srcp = pbk[:, 0:512].rearrange("p (k t) -> p k t", k=4)
                            if kq % 2 == 0:
                                fw.op("act", lambda e: e.activation(out=dst, in_=srcp, func=AF.Copy), reads=[bPS[bk]], **({"writes": [b_xeT[s]]} if first else {"pwrites": [b_xeT[s]]}))
                            else:
                                fw.op("dve", lambda e: e.tensor_copy(out=dst, in_=srcp), reads=[bPS[bk]], **({"writes": [b_xeT[s]]} if first else {"pwrites": [b_xeT[s]]}))

                def stQ(e_):
                    s = e_ % 2
                    for c in range(CT):
                        bg = 2 + 2 * (c % 2)
                        bu = bg + 1
                        for k in range(16):
                            mm(PS[bg][:, :], xeT[s][:, k, c * 128:(c + 1) * 128], wg[s][:, k, :], k == 0, k == 15, [b_wgu[s], b_xeT[s]], bPS[bg], k == 0)
                        for k in range(16):
                            mm(PS[bu][:, :], xeT[s][:, k, c * 128:(c + 1) * 128], wu[s][:, k, :], k == 0, k == 15, [b_wgu[s], b_xeT[s]], bPS[bu], k == 0)
                        q = cnt2["sg"] % 2
                        cnt2["sg"] += 1
                        fw.op("act", lambda e: e.activation(out=sgl[q][:], in_=PS[bg][:, :], func=AF.Silu), reads=[bPS[bg]], writes=[b_sgl[q]])
                        fw.op("dve", lambda e: e.tensor_tensor(out=hb[q][:], in0=sgl[q][:], in1=PS[bu][:, :], op=ALU.mult), reads=[b_sgl[q], bPS[bu]], writes=[b_hb[q]])
                        bk = c % 2
                        pbk = PS[bk][:, :].bitcast(BF16)
                        for fc in range(4):
                            fw.op("pe", lambda e: e.transpose(pbk[:, fc * 128:(fc + 1) * 128], hb[q][:, fc * 128:(fc + 1) * 128], ident_b[:]),
                                  reads=[b_hb[q], bC], **({"writes": [bPS[bk]]} if fc == 0 else {"pwrites": [bPS[bk]]}))
                        dst = hT[s][:, :, c * 128:(c + 1) * 128]
                        srcp = pbk[:, 0:512].rearrange("p (k t) -> p k t", k=4)
                        if c % 2 == 0:
                            fw.op("act", lambda e: e.activation(out=dst, in_=srcp, func=AF.Copy), reads=[bPS[bk]], **({"writes": [b_hT[s]]} if c == 0 else {"pwrites": [b_hT[s]]}))
                        else:
                            fw.op("dve", lambda e: e.tensor_copy(out=dst, in_=srcp), reads=[bPS[bk]], **({"writes": [b_hT[s]]} if c == 0 else {"pwrites": [b_hT[s]]}))

                def stR(e_):
                    s = e_ % 2
                    for c in range(CT):
                        q = cnt2["ye"] % 2
                        cnt2["ye"] += 1
                        for cb in range(4):
                            bk = 6 + cb % 2
                            for fc in range(4):
                                mm(PS[bk][:, :], hT[s][:, fc, c * 128:(c + 1) * 128], wdn[s][:, fc, cb * 512:(cb + 1) * 512], fc == 0, fc == 3, [b_hT[s], b_wd[s]], bPS[bk], fc == 0)
                            if cb % 2 == 0:
                                fw.op("act", lambda e: e.activation(out=ye[q][:, cb * 512:(cb + 1) * 512], in_=PS[bk][:, :], func=AF.Copy), reads=[bPS[bk]],
                                      **({"writes": [b_ye[q]]} if cb == 0 else {"pwrites": [b_ye[q]]}))
                            else:
                                fw.op("dve", lambda e: e.tensor_copy(out=ye[q][:, cb * 512:(cb + 1) * 512], in_=PS[bk][:, :]), reads=[bPS[bk]], pwrites=[b_ye[q]])
                        r0 = e_ * CAP + c * 128
                        fw.dma("sp", lambda e: e.dma_start(out=yd[r0:r0 + 128, :], in_=ye[q][:]), reads=[b_ye[q]], pwrites=[b_yd])

                load_x(0)
                load_gu(0)
                load_d(0)
                load_x(1)
                load_gu(1)
                load_d(1)
                for it in range(NE + 2):
                    if it < NE:
                        stP(it)
                        load_x(it + 2)
                    if 0 <= it - 1 < NE:
                        stQ(it - 1)
                        if it + 1 >= 2:
                            load_gu(it + 1)
                    if 0 <= it - 2 < NE:
                        stR(it - 2)
                        load_d(it)
                fw.barrier()

        def phase_combine(l, destI, wts, b_rt, dst, b_dst):
            with contextlib.ExitStack() as ph:
                g2 = sb(ph, "g2", [128, D], F32)
                b2 = sb(ph, "b2", [128, D], F32)
                b_gb = Buf("gb2")
                for t_, nm in ((g2, "ln2_g"), (b2, "ln2_b")):
                    fw.dma("sp", lambda e: e.dma_start(out=t_[:], in_=Wd[nm][l, :].partition_broadcast(128)), pwrites=[b_gb])
                x1t = [sb(ph, f"x1t{i}", [128, D], F32) for i in range(2)]
                b_x1t = [Buf(f"x1t{i}") for i in range(2)]
                rr = [[sb(ph, f"rr{i}{k}", [128, D], F32) for k in range(2)] for i in range(2)]
                b_rr = [[Buf(f"rr{i}{k}") for k in range(2)] for i in range(2)]
                stats = sb(ph, "stats2", [128, 4, 6], F32)
                mv = sb(ph, "mv2", [128, 4], F32)
                b_st = Buf("st2")
                for i in range(2):
                    for k in range(2):
                        fw.op("pool", lambda e: e.memset(rr[i][k][:], 0.0), writes=[b_rr[i][k]])
                for ti in range(NT):
                    s = ti % 2
                    r0 = ti * 128
                    fw.dma("sp", lambda e: e.dma_start(out=x1t[s][:], in_=x1d[r0:r0 + 128, :]), reads=[b_x1d], writes=[b_x1t[s]])
                    for k2 in range(2):
                        fw.dma("pool", lambda e: e.indirect_dma_start(out=rr[s][k2][:], out_offset=None, in_=yd[:, :],
                                                                      in_offset=bass.IndirectOffsetOnAxis(ap=destI[:, ti, k2:k2 + 1], axis=0),
                                                                      bounds_check=bc_reg, oob_is_err=False),
                               reads=[b_yd, b_rt], writes=[b_rr[s][k2]])
                    fw.op("act", lambda e: e.activation(out=x1t[s][:], in_=x1t[s][:], func=AF.Copy, scale=float(cfg.ALPHA)), reads=[b_x1t[s]], writes=[b_x1t[s]])
                    for k2 in range(2):
                        fw.op("dve", lambda e: e.scalar_tensor_tensor(out=x1t[s][:], in0=rr[s][k2][:], scalar=wts[:, ti, k2:k2 + 1], in1=x1t[s][:], op0=ALU.mult, op1=ALU.add),
                              reads=[b_rr[s][k2], b_rt, b_x1t[s]], writes=[b_x1t[s]])
                    layer_norm_tile(x1t[s], b_x1t[s], g2, b2, b_gb, stats, mv, b_st)
                    fw.dma("sp", lambda e: e.dma_start(out=dst[r0:r0 + 128, :], in_=x1t[s][:]), reads=[b_x1t[s]], pwrites=[b_dst])
                fw.barrier()

        xsrc, b_xsrc = x_in, b_xin
        for l in range(cfg.DEPTH):
            phase_inproj(l, xsrc, b_xsrc)
            if stop_after == "A":
                break
            phase_attention(l)
            if stop_after == "C":
                break
            with contextlib.ExitStack() as lay:
                destI = sb(lay, "destI", [128, NT, 2], I32)
                wts = sb(lay, "wts", [128, NT, 2], F32)
                b_rt = Buf("route")
                phase_outproj_router(l, xsrc, b_xsrc, destI, wts, b_rt)
                if stop_after == "D":
                    break
                phase_experts(l)
                if stop_after == "F":
                    break
                last = (l == cfg.DEPTH - 1)
                dst, b_dst = (y_out, b_yout) if last else (xmid, b_xmid)
                phase_combine(l, destI, wts, b_rt, dst, b_dst)
            xsrc, b_xsrc = xmid, b_xmid
        fw.barrier()
    return nc, hc, dbg, fw.ninstr


_CACHE = {}


def kernel(**inputs):
    cfg = Cfg(S=4096, DEPTH=2, NG=8, CAP=256)
    if "nc" not in _CACHE:
        _CACHE["nc"] = build(cfg)
    nc, hc, _, _ = _CACHE["nc"]
    x = np.asarray(inputs["x"], np.float32)
    B = x.shape[0]
    base = {}
    for k in WEIGHT_SHAPES(cfg):
        base[k] = np.ascontiguousarray(np.asarray(inputs[k], np.float32))
    for k, v in hc.items():
        base["c_" + k] = v
    in_maps = []
    for c in range(8):
        m = dict(base)
        m["x"] = np.ascontiguousarray(x[c % B])
        in_maps.append(m)
    res = run_bass_kernel_spmd(nc, in_maps, core_ids=list(range(8)))
    out = np.stack([np.asarray(res.results[b]["y"], np.float32) for b in range(B)], axis=0)
    return out
```

```python
import contextlib
import numpy as np
import ml_dtypes
import concourse.bass as bass
import concourse.mybir as mybir
from concourse.bass_utils import run_bass_kernel_spmd

F32 = mybir.dt.float32
BF16 = mybir.dt.bfloat16
I32 = mybir.dt.int32
U32 = mybir.dt.uint32
AF = mybir.ActivationFunctionType
ALU = mybir.AluOpType
AX = mybir.AxisListType
NEG = -30000.0


class Buf:
    __slots__ = ("name", "w", "r", "f")

    def __init__(self, name=""):
        self.name = name
        self.w = {}
        self.f = {}
        self.r = {}


class FW:
    EPOCH = 12000
    NDMA = 20

    def __init__(self, nc, es):
        self.nc = nc
        self.es = es
        self.eng = {"pe": nc.tensor, "act": nc.scalar, "dve": nc.vector, "pool": nc.gpsimd, "sp": nc.sync}
        self.cnt = {e: 0 for e in self.eng}
        self.csem = {}
        self.nsem = 0
        for e in ("pe", "act", "dve", "pool"):
            self.csem[e] = self._newsem(f"c_{e}")
        self.dsem = {}
        self.dcnt = {}
        for q in ("sp", "pool", "act"):
            self.dsem[q] = [self._newsem(f"d_{q}{i}") for i in range(self.NDMA)]
            self.dcnt[q] = 0
        self.known = {e: {} for e in self.eng}
        self.all_tokens = {}
        self.ninstr = 0

    def _newsem(self, name):
        self.nsem += 1
        return self.es.enter_context(self.nc.semaphore(f"{name}_{self.nsem}"))

    def _wait(self, e, tok):
        sem, val = tok
        k = id(sem)
        if self.known[e].get(k, 0) >= val:
            return
        self.eng[e].wait_ge(sem, val)
        self.known[e][k] = val

    def _deps(self, e, reads, writes, pwrites):
        for b in reads:
            for tok in b.w.values():
                self._wait(e, tok)
        for b in writes:
            for tok in b.w.values():
                self._wait(e, tok)
            for tok in b.r.values():
                self._wait(e, tok)
        for b in pwrites:
            for tok in b.f.values():
                self._wait(e, tok)
            for tok in b.r.values():
                self._wait(e, tok)

    def _record(self, tok, reads, writes, pwrites):
        sem, val = tok
        k = id(sem)
        self.all_tokens[k] = tok
        for b in reads:
            b.r[k] = tok
        for b in writes:
            b.w = {k: tok}
            b.f = {k: tok}
            b.r = {}
        for b in pwrites:
            b.w[k] = tok

    def op(self, e, fn, reads=(), writes=(), pwrites=()):
        self._deps(e, reads, writes, pwrites)
        if self.cnt[e] >= self.EPOCH:
            self.csem[e] = self._newsem(f"c_{e}")
            self.cnt[e] = 0
        ins = fn(self.eng[e])
        self.cnt[e] += 1
        sem = self.csem[e]
        ins.then_inc(sem, 1)
        tok = (sem, self.cnt[e])
        if e == "pe":
            self.known[e][id(sem)] = self.cnt[e]
        self._record(tok, reads, writes, pwrites)
        self.ninstr += 1
        return tok

    def dma(self, q, fn, reads=(), writes=(), pwrites=()):
        self._deps(q, reads, writes, pwrites)
        i = self.dcnt[q]
        sem = self.dsem[q][i % self.NDMA]
        rnd = i // self.NDMA
        if rnd > 0:
            self._wait(q, (sem, 16 * rnd))
        ins = fn(self.eng[q])
        ins.then_inc(sem, 16)
        self.dcnt[q] += 1
        tok = (sem, 16 * (rnd + 1))
        self._record(tok, reads, writes, pwrites)
        self.ninstr += 1
        return tok

    def barrier(self):
        for e in self.eng:
            for tok in list(self.all_tokens.values()):
                self._wait(e, tok)


class Cfg:
    def __init__(self, S=4096, DEPTH=2, NG=8, CAP=256, debug=False):
        self.S = S
        self.D = 2048
        self.DEPTH = DEPTH
        self.NG = NG
        self.EPG = 8
        self.NE = NG * 8
        self.CAP = CAP
        self.HID = 512
        self.NT = S // 128
        self.NQB = S // 512
        self.NCMP = (S - 32) // 16 + 1
        self.NSLC = S // 64
        self.KSEL = min(16, self.NSLC)
        self.ALPHA = (2.0 * 2) ** 0.25
        self.debug = debug
        self.branches = (0, 1, 2)
        self.look = 2
        self.c1skew = True


SRC_COLS = [i * 128 for i in range(8)] + [1280 + i * 128 for i in range(8)] + [1024, 2304, 2432, 2560, 2816]
NCHUNK = len(SRC_COLS)
ROW_KA, ROW_KC, ROW_VC, ROW_KS, ROW_KW = 2048, 2176, 2304, 2432, 2560


def _bf(x):
    return np.asarray(x, np.float32).astype(ml_dtypes.bfloat16)


def host_consts(cfg):
    S, NT, NCMP, NSLC = cfg.S, cfg.NT, cfg.NCMP, cfg.NSLC
    c = {}
    pos = np.arange(S)
    kaug = np.stack([pos // 64, pos % 64, np.ones(S), np.ones(S), np.ones(S)]).astype(np.float32)
    c["kaug"] = _bf(kaug)
    posc = 16 * np.arange(256) + 31
    kaugc = np.stack([posc // 64, posc % 64, np.ones(256), np.ones(256), np.ones(256)]).astype(np.float32)
    c["kaugc"] = _bf(kaugc)
    n = 32
    s_all = np.exp2(-8.0 * np.arange(1, n + 1, dtype=np.float32) / n).astype(np.float32)
    slopes = np.concatenate([s_all[0::2], s_all[1::2]])
    qaug = np.zeros((5, 32, S), np.float32)
    for h in range(32):
        ch = float(_bf(np.float32(8.0 * slopes[h])).astype(np.float32))
        A = -(np.float64(ch) * pos.astype(np.float64))
        a1 = _bf(A).astype(np.float64)
        a2 = _bf(A - a1).astype(np.float64)
        a3 = _bf(A - a1 - a2).astype(np.float64)
        qaug[0, h] = 64.0 * ch
        qaug[1, h] = ch
        qaug[2, h] = a1
        qaug[3, h] = a2
        qaug[4, h] = a3
    c["qaug"] = _bf(qaug)
    k = np.arange(128)[:, None]
    q = np.arange(128)[None, :]
    c["caus"] = _bf(np.where(k <= q, 0.0, NEG))
    c["anti"] = _bf(np.where(k > q, 0.0, NEG))
    q2 = np.arange(256)[None, :]
    c["swam"] = _bf(np.where((q2 - k >= 0) & (q2 - k < 128), 0.0, NEG))
    W = 8 * (NT - 1) + NCMP
    m = np.arange(W)[None, :]
    p = np.arange(128)[:, None]
    c["cmpm"] = _bf(np.where(16 * (m - 8 * (NT - 1)) + 31 <= p, 0.0, NEG))
    E = (np.arange(S)[None, :] // 64 == np.arange(NSLC)[:, None]).astype(np.float32)
    c["eblk"] = _bf(E)
    t = np.arange(S)
    cur = t // 64
    j = np.arange(NSLC)[None, :]
    forced = (j == 0) | (j == cur[:, None]) | (j == cur[:, None] - 1)
    valid = (j * 64) <= t[:, None]
    sb = np.where(valid, np.where(forced, 1e9, 0.0), -1e9).astype(np.float32)
    c["selbias"] = np.ascontiguousarray(sb.reshape(NT, 128, NSLC).transpose(1, 0, 2))
    c["identb"] = _bf(np.eye(128))
    c["identf"] = np.eye(128, dtype=np.float32)
    c["ltri"] = (np.arange(128)[:, None] < np.arange(128)[None, :]).astype(np.float32)
    c["onesf"] = np.ones((128, 128), np.float32)
    c["iotae"] = np.tile(np.arange(cfg.NE, dtype=np.float32)[None, :], (128, 1))
    return c


CONST_DT = {"kaug": BF16, "kaugc": BF16, "qaug": BF16, "caus": BF16, "anti": BF16, "swam": BF16, "cmpm": BF16,
            "eblk": BF16, "selbias": F32, "identb": BF16, "identf": F32, "ltri": F32, "onesf": F32, "iotae": F32}

WEIGHT_SHAPES = lambda cfg: {
    "w_in": [cfg.DEPTH, 2048, 3120], "b_in": [cfg.DEPTH, 3120], "swa_sinks": [cfg.DEPTH, 16],
    "cmp_pe_k": [cfg.DEPTH, 32, 64], "cmp_w1_k": [cfg.DEPTH, 2048, 256], "cmp_w2_k": [cfg.DEPTH, 256, 64],
    "cmp_pe_v": [cfg.DEPTH, 32, 64], "cmp_w1_v": [cfg.DEPTH, 2048, 256], "cmp_w2_v": [cfg.DEPTH, 256, 64],
    "w_out": [cfg.DEPTH, 2048, 2048], "b_out": [cfg.DEPTH, 2048], "ln1_g": [cfg.DEPTH, 2048], "ln1_b": [cfg.DEPTH, 2048],
    "w_group": [cfg.DEPTH, 2048, cfg.NG], "b_group": [cfg.DEPTH, cfg.NG],
    "w_expert": [cfg.DEPTH, 2048, cfg.NE], "b_expert": [cfg.DEPTH, cfg.NE],
    "we_gate": [cfg.DEPTH, cfg.NE, 2048, 512], "we_up": [cfg.DEPTH, cfg.NE, 2048, 512],
    "we_down": [cfg.DEPTH, cfg.NE, 512, 2048], "ln2_g": [cfg.DEPTH, 2048], "ln2_b": [cfg.DEPTH, 2048],
}


def build(cfg, stop_after=None):
    S, D, NT, NQB, NCMP, NSLC, NE, NG, CAP = cfg.S, cfg.D, cfg.NT, cfg.NQB, cfg.NCMP, cfg.NSLC, cfg.NE, cfg.NG, cfg.CAP
    nc = bass.Bass("TRN2", target_bir_lowering=False)
    top = contextlib.ExitStack()
    dbg = {}
    with top:
        fw = FW(nc, top)
        x_in = nc.dram_tensor("x", [S, D], F32, kind="ExternalInput").ap()
        Wd = {k: nc.dram_tensor(k, shp, F32, kind="ExternalInput").ap() for k, shp in WEIGHT_SHAPES(cfg).items()}
        hc = host_consts(cfg)
        Cd = {k: nc.dram_tensor("c_" + k, list(v.shape), CONST_DT[k], kind="ExternalInput").ap() for k, v in hc.items()}
        y_out = nc.dram_tensor("y", [S, D], F32, kind="ExternalOutput").ap()

        def scratch(name, shape, dt):
            if cfg.debug:
                t = nc.dram_tensor(name, shape, dt, kind="ExternalOutput").ap()
                dbg[name] = t
                return t
            return nc.dram_tensor(name, shape, dt).ap()

        projT = scratch("projT", [NCHUNK * 128, S], BF16)
        vtok = scratch("vtok", [S, 384], BF16)
        gates = scratch("gates", [S, 48], F32)
        otok = scratch("otok", [S, D], BF16)
        x1d = scratch("x1d", [S, D], F32)
        xmid = scratch("xmid", [S, D], F32)
        xd = scratch("xd", [NE * CAP, D], BF16)
        yd = scratch("yd", [NE * CAP, D], F32)
        b_projT, b_vtok, b_gates, b_otok, b_x1d, b_xmid, b_xd, b_yd = [Buf(n) for n in
                                                                      ("projT", "vtok", "gates", "otok", "x1d", "xmid", "xd", "yd")]
        if cfg.debug:
            scratch("kcdbg", [2, 69, 256], BF16)
            scratch("vcdbg", [2, 128, 2, 64], BF16)
        b_xin = Buf("xin")
        b_yout = Buf("yout")

        _uid = [0]

        def sb(es, name, shape, dt):
            _uid[0] += 1
            return es.enter_context(nc.sbuf_tensor(f"{name}_{_uid[0]}", shape, dt))

        PS = [top.enter_context(nc.psum_tensor(f"ps{i}", [128, 512], F32)) for i in range(8)]
        bPS = [Buf(f"ps{i}") for i in range(8)]

        bc_reg = nc.gpsimd.to_reg(NE * CAP - 1)
        ident_b = sb(top, "ident_b", [128, 128], BF16)
        ident_f = sb(top, "ident_f", [128, 128], F32)
        bC = Buf("consts")
        fw.dma("sp", lambda e: e.dma_start(out=ident_b[:], in_=Cd["identb"][:, :]), pwrites=[bC])
        fw.dma("sp", lambda e: e.dma_start(out=ident_f[:], in_=Cd["identf"][:, :]), pwrites=[bC])

        def mm(out, lhsT, rhs, start, stop, reads, bank, first):
            if first:
                fw.op("pe", lambda e: e.matmul(out, lhsT, rhs, start=start, stop=stop), reads=reads, writes=[bank])
            else:
                fw.op("pe", lambda e: e.matmul(out, lhsT, rhs, start=start, stop=stop), reads=reads, pwrites=[bank])

        def phase_inproj(l, xsrc, b_xsrc):
            with contextlib.ExitStack() as ph:
                w_sb = sb(ph, "w_in_sb", [128, 16, 3120], BF16)
                b_w = Buf("w_in")
                wsrc = Wd["w_in"][l].rearrange("(k p) c -> p k c", p=128)
                for c0 in (0, 1560):
                    fw.dma("pool", lambda e: e.dma_start(out=w_sb[:, :, c0:c0 + 1560], in_=wsrc[:, :, c0:c0 + 1560]), pwrites=[b_w])
                bias_fm = sb(ph, "bias_fm", [128, NCHUNK], F32)
                b_bias = Buf("bias")
                with nc.allow_non_contiguous_dma(reason="tiny bias column loads"):
                    for ci, c0 in enumerate(SRC_COLS):
                        fw.dma("sp", lambda e: e.dma_start(out=bias_fm[:, ci:ci + 1],
                                                           in_=Wd["b_in"][l, c0:c0 + 128].rearrange("(p o) -> p o", o=1)), pwrites=[b_bias])
                bias_tm = sb(ph, "bias_tm", [128, 432], F32)
                for (d0, s0, n) in ((0, 1152, 128), (128, 2688, 128), (256, 2944, 176)):
                    fw.dma("sp", lambda e: e.dma_start(out=bias_tm[:, d0:d0 + n], in_=Wd["b_in"][l, s0:s0 + n].partition_broadcast(128)), pwrites=[b_bias])
                xt = [sb(ph, f"xt{i}", [128, D], F32) for i in range(2)]
                b_xt = [Buf(f"xt{i}") for i in range(2)]
                xT = sb(ph, "xT", [128, 16, 512], BF16)
                b_xT = Buf("xT")
                stg = [sb(ph, f"stg{i}", [128, 512], BF16) for i in range(2)]
                b_stg = [Buf(f"stg{i}") for i in range(2)]
                vst = [sb(ph, f"vst{i}", [128, 384], BF16) for i in range(2)]
                b_vst = [Buf(f"vst{i}") for i in range(2)]
                gst = [sb(ph, f"gst{i}", [128, 48], F32) for i in range(2)]
                b_gst = [Buf(f"gst{i}") for i in range(2)]
                gtmp = sb(ph, "gtmp", [128, 48], F32)
                b_gtmp = Buf("gtmp")
                nload = 0
                nstg = 0
                nv = 0
                for tb in range(NQB):
                    for j in range(4):
                        ti = tb * 4 + j
                        s = nload % 2
                        nload += 1
                        fw.dma("sp", lambda e: e.dma_start(out=xt[s][:], in_=xsrc[ti * 128:(ti + 1) * 128, :]), reads=[b_xsrc], writes=[b_xt[s]])
                        for kq in range(4):
                            bk = (j * 4 + kq) % 4
                            for kk in range(4):
                                k = kq * 4 + kk
                                fw.op("pe", lambda e: e.transpose(PS[bk][:, kk * 128:(kk + 1) * 128], xt[s][:, k * 128:(k + 1) * 128], ident_f[:]),
                                      reads=[b_xt[s], bC], **({"writes": [bPS[bk]]} if kk == 0 else {"pwrites": [bPS[bk]]}))
                            src = PS[bk][:, :].rearrange("p (k t) -> p k t", k=4)
                            dst = xT[:, kq * 4:(kq + 1) * 4, j * 128:(j + 1) * 128]
                            if kq % 2 == 0:
                                fw.op("act", lambda e: e.activation(out=dst, in_=src, func=AF.Copy), reads=[bPS[bk]], pwrites=[b_xT])
                            else:
                                fw.op("dve", lambda e: e.tensor_copy(out=dst, in_=src), reads=[bPS[bk]], pwrites=[b_xT])
                    for ci, c0 in enumerate(SRC_COLS):
                        bk = 4 + (ci % 2)
                        for k in range(16):
                            mm(PS[bk][:, :], w_sb[:, k, c0:c0 + 128], xT[:, k, :], k == 0, k == 15, [b_w, b_xT], bPS[bk], k == 0)
                        s = nstg % 2
                        nstg += 1
                        fw.op("act", lambda e: e.activation(out=stg[s][:], in_=PS[bk][:, :], func=AF.Identity, bias=bias_fm[:, ci:ci + 1], scale=1.0),
                              reads=[bPS[bk], b_bias], writes=[b_stg[s]])
                        fw.dma("sp", lambda e: e.dma_start(out=projT[ci * 128:(ci + 1) * 128, tb * 512:(tb + 1) * 512], in_=stg[s][:]),
                               reads=[b_stg[s]], pwrites=[b_projT])
                    for j in range(4):
                        ti = tb * 4 + j
                        bk = 6 + (j % 2)
                        first = True
                        for (d0, s0, n) in ((0, 1152, 128), (128, 2688, 128), (256, 2944, 176)):
                            for k in range(16):
                                mm(PS[bk][:, d0:d0 + n], xT[:, k, j * 128:(j + 1) * 128], w_sb[:, k, s0:s0 + n], k == 0, k == 15,
                                   [b_w, b_xT], bPS[bk], first)
                                first = False
                        s = nv % 2
                        nv += 1
                        fw.op("dve", lambda e: e.tensor_tensor(out=vst[s][:], in0=PS[bk][:, 0:384], in1=bias_tm[:, 0:384], op=ALU.add),
                              reads=[bPS[bk], b_bias], writes=[b_vst[s]])
                        fw.op("dve", lambda e: e.tensor_tensor(out=gtmp[:], in0=PS[bk][:, 384:432], in1=bias_tm[:, 384:432], op=ALU.add),
                              reads=[bPS[bk], b_bias], writes=[b_gtmp])
                        fw.op("act", lambda e: e.activation(out=gst[s][:], in_=gtmp[:], func=AF.Sigmoid), reads=[b_gtmp], writes=[b_gst[s]])
                        fw.dma("sp", lambda e: e.dma_start(out=vtok[ti * 128:(ti + 1) * 128, :], in_=vst[s][:]), reads=[b_vst[s]], pwrites=[b_vtok])
                        fw.dma("sp", lambda e: e.dma_start(out=gates[ti * 128:(ti + 1) * 128, :], in_=gst[s][:]), reads=[b_gst[s]], pwrites=[b_gates])
                fw.barrier()

        NTILES_C = [(0, min(128, NCMP))] + ([(128, NCMP - 128)] if NCMP > 128 else [])

        def phase_compress(l, kcT, vc, b_kc):
            with contextlib.ExitStack() as ph:
                w1 = sb(ph, "cw1", [64, 32, 256], BF16)
                w2 = sb(ph, "cw2", [128, 2, 64], BF16)
                peT = sb(ph, "cpeT", [64, 32], F32)
                kvT = sb(ph, "ckvT", [64, S], BF16)
                kvpe = sb(ph, "ckvpe", [64, 32, 256], BF16)
                hT = sb(ph, "chT", [128, 2, 256], BF16)
                xs = sb(ph, "cxs", [128, 256], F32)
                t1 = sb(ph, "ct1", [128, 256], F32)
                sg = sb(ph, "csg", [128, 256], F32)
                b_w, b_kvT, b_kvpe, b_hT, b_xs, b_t1, b_sg = [Buf(n) for n in ("cw", "kvT", "kvpe", "hT", "xs", "t1", "sg")]
                for kv in (0, 1):
                    sfx = "k" if kv == 0 else "v"
                    fw.dma("pool", lambda e: e.dma_start(out=w1[:], in_=Wd["cmp_w1_" + sfx][l].rearrange("(l d) h -> d l h", d=64)), writes=[b_w])
                    fw.dma("pool", lambda e: e.dma_start(out=w2[:], in_=Wd["cmp_w2_" + sfx][l].rearrange("(c p) o -> p c o", p=128)), pwrites=[b_w])
                    with nc.allow_non_contiguous_dma(reason="tiny pe transpose load"):
                        fw.dma("sp", lambda e: e.dma_start(out=peT[:], in_=Wd["cmp_pe_" + sfx][l].rearrange("l d -> d l")), pwrites=[b_w])
                    for g in (0, 1):
                        row0 = (ROW_KC if kv == 0 else ROW_VC) + g * 64
                        fw.dma("sp", lambda e: e.dma_start(out=kvT[:], in_=projT[row0:row0 + 64, :]), reads=[b_projT], writes=[b_kvT])
                        for ll in range(32):
                            fw.op("act", lambda e: e.activation(out=kvpe[:, ll, 0:NCMP], in_=kvT[:, ll:ll + 16 * (NCMP - 1) + 1:16],
                                                                func=AF.Identity, bias=peT[:, ll:ll + 1], scale=1.0),
                                  reads=[b_kvT, b_w], **({"writes": [b_kvpe]} if ll == 0 else {"pwrites": [b_kvpe]}))
                        for hc2 in (0, 1):
                            for ll in range(32):
                                mm(PS[hc2][:, 0:NCMP], w1[:, ll, hc2 * 128:(hc2 + 1) * 128], kvpe[:, ll, 0:NCMP], ll == 0, ll == 31,
                                   [b_w, b_kvpe], bPS[hc2], ll == 0)
                            fw.op("act", lambda e: e.activation(out=xs[:, 0:NCMP], in_=PS[hc2][:, 0:NCMP], func=AF.Copy), reads=[bPS[hc2]], writes=[b_xs])
                            fw.op("dve", lambda e: e.tensor_tensor(out=t1[:, 0:NCMP], in0=xs[:, 0:NCMP], in1=xs[:, 0:NCMP], op=ALU.mult), reads=[b_xs], writes=[b_t1])
                            fw.op("dve", lambda e: e.tensor_scalar(out=t1[:, 0:NCMP], in0=t1[:, 0:NCMP], scalar1=0.044715, scalar2=1.0, op0=ALU.mult, op1=ALU.add),
                                  reads=[b_t1], writes=[b_t1])
                            fw.op("dve", lambda e: e.tensor_tensor(out=t1[:, 0:NCMP], in0=t1[:, 0:NCMP], in1=xs[:, 0:NCMP], op=ALU.mult), reads=[b_t1, b_xs], writes=[b_t1])
                            fw.op("act", lambda e: e.activation(out=sg[:, 0:NCMP], in_=t1[:, 0:NCMP], func=AF.Sigmoid, scale=1.5957691216057308),
                                  reads=[b_t1], writes=[b_sg])
                            fw.op("dve", lambda e: e.tensor_tensor(out=hT[:, hc2, 0:NCMP], in0=xs[:, 0:NCMP], in1=sg[:, 0:NCMP], op=ALU.mult),
                                  reads=[b_xs, b_sg], **({"writes": [b_hT]} if hc2 == 0 else {"pwrites": [b_hT]}))
                        if kv == 0:
                            for hc2 in (0, 1):
                                mm(PS[2][0:64, 0:NCMP], w2[:, hc2, :], hT[:, hc2, 0:NCMP], hc2 == 0, hc2 == 1, [b_w, b_hT], bPS[2], hc2 == 0)
                            fw.op("act", lambda e: e.activation(out=kcT[g][0:64, 0:NCMP], in_=PS[2][0:64, 0:NCMP], func=AF.Copy), reads=[bPS[2]], pwrites=[b_kc])
                        else:
                            for nti, (n0, rows) in enumerate(NTILES_C):
                                for hc2 in (0, 1):
                                    mm(PS[3][0:rows, nti * 64:(nti + 1) * 64], hT[:, hc2, n0:n0 + rows], w2[:, hc2, :], hc2 == 0, hc2 == 1,
                                       [b_w, b_hT], bPS[3], (nti == 0 and hc2 == 0))
                                fw.op("act", lambda e: e.activation(out=vc[g][0:rows, nti, :], in_=PS[3][0:rows, nti * 64:(nti + 1) * 64], func=AF.Copy),
                                      reads=[bPS[3]], pwrites=[b_kc])
                fw.barrier()

        def phase_attention(l):
            with contextlib.ExitStack() as ph:
                kcT = [sb(ph, f"kcT{g}", [69, 256], BF16) for g in (0, 1)]
                vc = [sb(ph, f"vc{g}", [128, 2, 64], BF16) for g in (0, 1)]
                b_kc = Buf("kc")
                for g in (0, 1):
                    fw.dma("sp", lambda e: e.dma_start(out=kcT[g][64:69, :], in_=Cd["kaugc"][:, :]), pwrites=[b_kc])
                phase_compress(l, kcT, vc, b_kc)
                if cfg.debug:
                    for g in (0, 1):
                        fw.dma("sp", lambda e: e.dma_start(out=dbg["kcdbg"][g], in_=kcT[g][:, :]), reads=[b_kc])
                        fw.dma("sp", lambda e: e.dma_start(out=dbg["vcdbg"][g], in_=vc[g][:, :, :]), reads=[b_kc])
                b_K = Buf("KV")
                KT = {}
                VV = {}
                for nm, row0, voff in (("a", ROW_KA, 0), ("s", ROW_KS, 128), ("w", ROW_KW, 256)):
                    for g in (0, 1):
                        kt_ = sb(ph, f"KT{nm}{g}", [69, S], BF16)
                        vv_ = sb(ph, f"VV{nm}{g}", [128, NT, 65], BF16)
                        KT[nm, g] = kt_
                        VV[nm, g] = vv_
                        r0 = row0 + g * 64
                        fw.dma("sp", lambda e: e.dma_start(out=kt_[0:64, :], in_=projT[r0:r0 + 64, :]), reads=[b_projT], pwrites=[b_K])
                        fw.dma("sp", lambda e: e.dma_start(out=kt_[64:69, :], in_=Cd["kaug"][:, :]), pwrites=[b_K])
                        c0 = voff + g * 64
                        with nc.allow_non_contiguous_dma(reason="v token-major tiles (128B rows)"):
                            fw.dma("sp", lambda e: e.dma_start(out=vv_[:, :, 0:64], in_=vtok[:, c0:c0 + 64].rearrange("(t p) c -> p t c", p=128)),
                                   reads=[b_vtok], pwrites=[b_K])
                        fw.op("dve", lambda e: e.memset(vv_[:, :, 64:65], 1.0), pwrites=[b_K])
                caus = sb(ph, "caus", [128, 128], BF16)
                anti = sb(ph, "anti", [128, 128], BF16)
                swam = sb(ph, "swam", [128, 256], BF16)
                WCM = 8 * (NT - 1) + NCMP
                cmpm = sb(ph, "cmpm", [128, WCM], BF16)
                eblk = sb(ph, "eblk", [NSLC, S], BF16)
                selb = sb(ph, "selb", [128, NT, NSLC], F32)
                esink = sb(ph, "esink", [128, 16], F32)
                for t_, nm in ((caus, "caus"), (anti, "anti"), (swam, "swam"), (cmpm, "cmpm"), (eblk, "eblk")):
                    fw.dma("sp", lambda e: e.dma_start(out=t_[:], in_=Cd[nm][:, :]), pwrites=[b_K])
                fw.dma("sp", lambda e: e.dma_start(out=selb[:], in_=Cd["selbias"][:, :, :]), pwrites=[b_K])
                b_es = Buf("esink")
                fw.dma("sp", lambda e: e.dma_start(out=esink[:], in_=Wd["swa_sinks"][l, :].partition_broadcast(128)), writes=[b_es])
                fw.op("act", lambda e: e.activation(out=esink[:], in_=esink[:], func=AF.Exp), reads=[b_es], writes=[b_es])

                QT = sb(ph, "QT", [69, 32, 512], BF16)
                b_QT = Buf("QT")
                gt = sb(ph, "gt", [128, 4, 48], F32)
                b_gt = Buf("gt")
                ofp = sb(ph, "ofp", [128, 4, 16, 64], F32)
                b_ofp = Buf("ofp")
                otile = sb(ph, "otile", [128, 4, 2048], BF16)
                b_ot = Buf("otile")
                PT = [sb(ph, f"PT{i}", [128, 512], BF16) for i in range(3)]
                b_PT = [Buf(f"PT{i}") for i in range(3)]
                nselT = [sb(ph, f"nselT{g}", [NSLC, 512], BF16) for g in (0, 1)]
                b_ns = [Buf(f"nselT{g}") for g in (0, 1)]
                pcs = sb(ph, "pcs", [128, 4 * NSLC + 4], F32)
                b_pcs = Buf("pcs")
                pf = [sb(ph, f"pf{i}", [128, 256], F32) for i in range(2)]
                b_pf = [Buf(f"pf{i}") for i in range(2)]
                pcb = [sb(ph, f"pcb{i}", [128, 256], BF16) for i in range(2)]
                b_pcb = [Buf(f"pcb{i}") for i in range(2)]
                pcT = [sb(ph, f"pcT{i}", [128, 2, 128], BF16) for i in range(2)]
                b_pcT = [Buf(f"pcT{i}") for i in range(2)]
                den = [sb(ph, f"den{i}", [128, 8], F32) for i in range(4)]
                b_den = [Buf(f"den{i}") for i in range(4)]
                imp = sb(ph, "imp", [128, NSLC], F32)
                sc2 = sb(ph, "sc2", [128, NSLC], F32)
                m8 = sb(ph, "m8", [128, 16], F32)
                msk = sb(ph, "msk", [128, 2, NSLC], F32)
                nsel = sb(ph, "nsel", [128, NSLC], BF16)
                b_tk = Buf("topk")
                cnt = {"st": 0, "acc": 0, "pt": 0, "c1": 0, "den": 0}

                def st_tile():
                    i = cnt["st"] % 3
                    cnt["st"] += 1
                    return PS[i], bPS[i]

                def acc_tile():
                    i = 3 + cnt["acc"] % 2
                    cnt["acc"] += 1
                    return PS[i], bPS[i]

                def pt_tile():
                    i = cnt["pt"] % 3
                    cnt["pt"] += 1
                    return PT[i], b_PT[i]

                def den_tile():
                    i = cnt["den"] % 4
                    cnt["den"] += 1
                    return den[i], b_den[i]

                S5 = [PS[5], PS[0]]
                bS5 = [bPS[5], bPS[0]]
                P6 = [PS[6][:, :].bitcast(BF16), PS[1][:, :].bitcast(BF16)]
                bP6 = [bPS[6], bPS[1]]
                P7 = [PS[7], PS[2]]
                bP7 = [bPS[7], bPS[2]]
                P7n = PS[3][:, :].bitcast(BF16)
                bP7n = bPS[3]
                pb = [sb(ph, f"pbb{i}", [128, 256], BF16) for i in range(2)]
                b_pb = [Buf(f"pbb{i}") for i in range(2)]
                den8 = [sb(ph, f"den8_{i}", [128, 8], F32) for i in range(8)]
                b_den8 = [Buf(f"den8_{i}") for i in range(8)]
                LOOK = cfg.look

                class Job:
                    __slots__ = ("s1", "s2")

                def make_job(H, ktile_ap, c0, c1, masks, vtile_ap, acc, bacc, subtiles, pv_state, extra_reads, fin):
                    jb = Job()
                    hold = {}

                    def s1():
                        st, bst = st_tile()
                        nm = len(masks)
                        mm(st[:, c0:c1], ktile_ap, QT[:, H, c0:c1], True, nm == 0, [b_K, b_QT] + extra_reads, bst, True)
                        for mi, (lhsT, rhs, m0, m1) in enumerate(masks):
                            mm(st[:, m0:m1], lhsT, rhs, False, mi == nm - 1, [b_K, bC] + extra_reads, bst, False)
                        pt, bpt = pt_tile()
                        fw.op("act", lambda e: e.activation(out=pt[:, c0:c1], in_=st[:, c0:c1], func=AF.Exp, scale=0.125), reads=[bst], writes=[bpt])
                        hold["pt"] = (pt, bpt)

                    def s2():
                        pt, bpt = hold["pt"]
                        for j in subtiles:
                            first = pv_state["first"]
                            pv_state["first"] = False
                            pv_state["n"] -= 1
                            last = pv_state["n"] == 0
                            mm(acc[:, j * 128:j * 128 + 65], pt[:, j * 128:(j + 1) * 128], vtile_ap, first, last, [bpt, b_K], bacc, first)
                        if fin is not None and pv_state["n"] == 0:
                            fin()

                    jb.s1 = s1
                    jb.s2 = s2
                    return jb

                def run_jobs(jobs):
                    n = len(jobs)
                    for i in range(n + LOOK):
                        if i < n:
                            jobs[i].s1()
                        if i - LOOK >= 0:
                            jobs[i - LOOK].s2()

                for qb in range(NQB):
                    q0 = qb * 512
                    fw.dma("sp", lambda e: e.dma_start(out=QT[0:64, :, :], in_=projT[0:2048, q0:q0 + 512].rearrange("(h d) s -> d h s", d=64)),
                           reads=[b_projT], writes=[b_QT])
                    fw.dma("sp", lambda e: e.dma_start(out=QT[64:69, :, :], in_=Cd["qaug"][:, :, q0:q0 + 512]), pwrites=[b_QT])
                    fw.dma("sp", lambda e: e.dma_start(out=gt[:], in_=gates[q0:q0 + 512, :].rearrange("(j p) c -> p j c", p=128)), reads=[b_gates], writes=[b_gt])
                    ofp_state = {"first": True, "init": set()}

                    def ofp_write(j, hn, in0_ap, scal_ap, reads):
                        if (j, hn) in ofp_state["init"]:
                            fw.op("dve", lambda e: e.scalar_tensor_tensor(out=ofp[:, j, hn, :], in0=in0_ap, scalar=scal_ap, in1=ofp[:, j, hn, :],
                                                                          op0=ALU.mult, op1=ALU.add), reads=reads, pwrites=[b_ofp])
                        else:
                            fw.op("dve", lambda e: e.tensor_scalar(out=ofp[:, j, hn, :], in0=in0_ap, scalar1=scal_ap, scalar2=None, op0=ALU.mult),
                                  reads=reads, **({"writes": [b_ofp]} if ofp_state["first"] else {"pwrites": [b_ofp]}))
                            ofp_state["first"] = False
                            ofp_state["init"].add((j, hn))

                    p6 = PS[6][:, :].bitcast(BF16)
                    p7 = PS[7][:, :].bitcast(BF16)
                    for g in (0, 1):
                        for j in range(4):
                            qt = qb * 4 + j
                            moff = 8 * (NT - 1 - qt)
                            fw.op("dve", lambda e: e.memset(pcs[:], 0.0), writes=[b_pcs])
                            dnr = {}

                            def stA(r):
                                hn = g * 8 + r
                                H = 16 + hn
                                a = r % 2
                                reg = S5[a][:, 0:NCMP]
                                mm(reg, QT[:, H, j * 128:(j + 1) * 128], kcT[g][:, 0:NCMP], True, False, [b_QT, b_kc], bS5[a], True)
                                mm(reg, ident_b[:], cmpm[:, moff:moff + NCMP], False, True, [bC, b_K], bS5[a], False)
                                i8 = cnt["den"] % 8
                                cnt["den"] += 1
                                dn, bdn = den8[i8], b_den8[i8]
                                dnr[r] = (dn, bdn)
                                fw.op("act", lambda e: e.activation(out=pb[a][:, 0:NCMP], in_=reg, func=AF.Exp, scale=0.125),
                                      reads=[bS5[a]], writes=[b_pb[a]])
                                fw.op("dve", lambda e: e.tensor_reduce(out=dn[:, 0:1], in_=pb[a][:, 0:NCMP], axis=AX.X, op=ALU.add), reads=[b_pb[a]], writes=[bdn])
                                fw.op("dve", lambda e: e.tensor_scalar(out=dn[:, 1:2], in0=dn[:, 0:1], scalar1=1e-30, scalar2=None, op0=ALU.max), reads=[bdn], writes=[bdn])
                                fw.op("dve", lambda e: e.reciprocal(out=dn[:, 2:3], in_=dn[:, 1:2]), reads=[bdn], writes=[bdn])
                                fw.op("dve", lambda e: e.tensor_tensor(out=dn[:, 3:4], in0=dn[:, 2:3], in1=gt[:, j, hn * 3:hn * 3 + 1], op=ALU.mult), reads=[bdn, b_gt], writes=[bdn])
                                fw.op("dve", lambda e: e.scalar_tensor_tensor(out=pcs[:, 1:1 + NCMP], in0=pb[a][:, 0:NCMP], scalar=dn[:, 2:3], in1=pcs[:, 1:1 + NCMP],
                                                                              op0=ALU.mult, op1=ALU.add), reads=[b_pb[a], bdn], writes=[b_pcs])

                            def stC(r):
                                a = r % 2
                                for nti, (n0, rows) in enumerate(NTILES_C):
                                    fw.op("pe", lambda e: e.transpose(P6[a][0:rows, nti * 128:(nti + 1) * 128], pb[a][:, n0:n0 + rows], ident_b[:]),
                                          reads=[b_pb[a], bC], **({"writes": [bP6[a]]} if nti == 0 else {"pwrites": [bP6[a]]}))
                                for nti, (n0, rows) in enumerate(NTILES_C):
                                    fw.op("act", lambda e: e.activation(out=pcT[a][0:rows, nti, :], in_=P6[a][0:rows, nti * 128:(nti + 1) * 128], func=AF.Copy),
                                          reads=[bP6[a]], **({"writes": [b_pcT[a]]} if nti == 0 else {"pwrites": [b_pcT[a]]}))

                            def stD(r):
                                hn = g * 8 + r
                                a = r % 2
                                dn, bdn = dnr[r]
                                reg = P7[a][:, 0:64]
                                for nti, (n0, rows) in enumerate(NTILES_C):
                                    mm(reg, pcT[a][0:rows, nti, :], vc[g][0:rows, nti, :], nti == 0, nti == len(NTILES_C) - 1,
                                       [b_pcT[a], b_kc], bP7[a], nti == 0)
                                if 0 in cfg.branches:
                                    ofp_write(j, hn, reg, dn[:, 3:4], [bP7[a], bdn])

                            if cfg.c1skew:
                                for step in range(8 + 2):
                                    if step < 8:
                                        stA(step)
                                    if 0 <= step - 1 < 8:
                                        stC(step - 1)
                                    if 0 <= step - 2 < 8:
                                        stD(step - 2)
                            else:
                                for step in range(8):
                                    stA(step)
                                    stC(step)
                                    stD(step)
                            fw.op("dve", lambda e: e.tensor_reduce(out=imp[:], in_=pcs[:, 0:4 * NSLC].rearrange("p (j i) -> p j i", i=4), axis=AX.X, op=ALU.add),
                                  reads=[b_pcs], writes=[b_tk])
                            fw.op("dve", lambda e: e.tensor_tensor(out=imp[:], in0=imp[:], in1=pcs[:, 4:4 * NSLC + 1:4], op=ALU.add), reads=[b_pcs, b_tk], writes=[b_tk])
                            fw.op("dve", lambda e: e.tensor_tensor(out=imp[:], in0=imp[:], in1=selb[:, qt, :], op=ALU.add), reads=[b_K, b_tk], writes=[b_tk])
                            fw.op("dve", lambda e: e.max(out=m8[:, 0:8], in_=imp[:]), reads=[b_tk], writes=[b_tk])
                            if cfg.KSEL == 16:
                                fw.op("dve", lambda e: e.match_replace(out=sc2[:], in_to_replace=m8[:, 0:8], in_values=imp[:], imm_value=-3e9), reads=[b_tk], writes=[b_tk])
                                fw.op("dve", lambda e: e.max(out=m8[:, 8:16], in_=sc2[:]), reads=[b_tk], writes=[b_tk])
                                thr = m8[:, 15:16]
                            else:
                                thr = m8[:, 7:8]
                            fw.op("dve", lambda e: e.tensor_scalar(out=msk[:, 0, :], in0=imp[:], scalar1=thr, scalar2=None, op0=ALU.is_ge), reads=[b_tk], writes=[b_tk])
                            fw.op("dve", lambda e: e.tensor_scalar(out=msk[:, 1, :], in0=imp[:], scalar1=-5e8, scalar2=None, op0=ALU.is_gt), reads=[b_tk], writes=[b_tk])
                            fw.op("dve", lambda e: e.tensor_tensor(out=msk[:, 0, :], in0=msk[:, 0, :], in1=msk[:, 1, :], op=ALU.mult), reads=[b_tk], writes=[b_tk])
                            fw.op("dve", lambda e: e.tensor_scalar(out=nsel[:], in0=msk[:, 0, :], scalar1=1.0, scalar2=-NEG, op0=ALU.subtract, op1=ALU.mult),
                                  reads=[b_tk], writes=[b_tk])
                            fw.op("pe", lambda e: e.transpose(P7n[0:NSLC, 0:128], nsel[:], ident_b[:]), reads=[b_tk, bC], writes=[bP7n])
                            fw.op("act", lambda e: e.activation(out=nselT[g][:, j * 128:(j + 1) * 128], in_=P7n[0:NSLC, 0:128], func=AF.Copy),
                                  reads=[bP7n], **({"writes": [b_ns[g]]} if j == 0 else {"pwrites": [b_ns[g]]}))
                    jobs = []
                    ot_state = {"first": True}
                    for g in (0, 1):
                        for r in range(8):
                            hn = g * 8 + r
                            H = 16 + hn
                            for br in (2, 1):
                                if br not in cfg.branches:
                                    continue
                                acc, bacc = acc_tile()
                                tiles = []
                                if br == 2:
                                    for i in range(4):
                                        kt = qb * 4 - 4 + i
                                        if kt >= 0:
                                            tiles.append((kt, 0, 128 * (i + 1), [(ident_b[:], anti[:], 128 * i, 128 * (i + 1))], list(range(0, i + 1))))
                                    for i in range(4):
                                        kt = qb * 4 + i
                                        tiles.append((kt, 128 * i, 512, [(ident_b[:], caus[:], 128 * i, 128 * (i + 1))], list(range(i, 4))))
                                    ktn, vvn = KT["w", g], VV["w", g]
                                else:
                                    for kt in range(qb * 4 + 4):
                                        i = kt - qb * 4
                                        c0 = 0 if i < 0 else 128 * i
                                        ms = [(eblk[:, kt * 128:(kt + 1) * 128], nselT[g][:, c0:512], c0, 512)]
                                        if i >= 0:
                                            ms.append((ident_b[:], caus[:], c0, c0 + 128))
                                        tiles.append((kt, c0, 512, ms, list(range(max(i, 0), 4))))
                                    ktn, vvn = KT["s", g], VV["s", g]
                                pv_state = {"first": True, "n": sum(len(t[4]) for t in tiles)}

                                def fin_nsa(acc=acc, bacc=bacc, hn=hn, br=br):
                                    dn, bdn = den_tile()
                                    accv = acc[:, :].rearrange("p (j c) -> p j c", j=4)
                                    fw.op("dve", lambda e: e.tensor_scalar(out=dn[:, 0:4], in0=accv[:, :, 64], scalar1=1e-30, scalar2=None, op0=ALU.max), reads=[bacc], writes=[bdn])
                                    fw.op("dve", lambda e: e.reciprocal(out=dn[:, 0:4], in_=dn[:, 0:4]), reads=[bdn], writes=[bdn])
                                    fw.op("dve", lambda e: e.tensor_tensor(out=dn[:, 4:8], in0=dn[:, 0:4], in1=gt[:, :, hn * 3 + br], op=ALU.mult), reads=[bdn, b_gt], writes=[bdn])
                                    for j in range(4):
                                        ofp_write(j, hn, accv[:, j, 0:64], dn[:, 4 + j:5 + j], [bacc, bdn])

                                for ti_, (kt, c0, c1, ms, subs) in enumerate(tiles):
                                    jobs.append(make_job(H, ktn[:, kt * 128:(kt + 1) * 128], c0, c1, ms, vvn[:, kt, :], acc, bacc, subs, pv_state,
                                                         [b_ns[g]] if br == 1 else [], fin_nsa))
                    for g in (0, 1):
                        for r in range(8):
                            H = g * 8 + r
                            acc, bacc = acc_tile()
                            tiles = []
                            kt = qb * 4 - 1
                            if kt >= 0:
                                tiles.append((kt, 0, 128, [(ident_b[:], anti[:], 0, 128)], [0]))
                            for i in range(4):
                                kt = qb * 4 + i
                                c1 = min(512, 128 * (i + 2))
                                tiles.append((kt, 128 * i, c1, [(ident_b[:], swam[:, 0:c1 - 128 * i], 128 * i, c1)], list(range(i, min(i + 2, 4)))))
                            pv_state = {"first": True, "n": sum(len(t[4]) for t in tiles)}

                            def fin_swa(acc=acc, bacc=bacc, H=H):
                                dn, bdn = den_tile()
                                accv = acc[:, :].rearrange("p (j c) -> p j c", j=4)
                                fw.op("dve", lambda e: e.tensor_scalar(out=dn[:, 0:4], in0=accv[:, :, 64], scalar1=esink[:, H:H + 1], scalar2=None, op0=ALU.add),
                                      reads=[bacc, b_es], writes=[bdn])
                                fw.op("dve", lambda e: e.reciprocal(out=dn[:, 0:4], in_=dn[:, 0:4]), reads=[bdn], writes=[bdn])
                                for j in range(4):
                                    fw.op("dve", lambda e: e.tensor_scalar(out=otile[:, j, H * 64:(H + 1) * 64], in0=accv[:, j, 0:64], scalar1=dn[:, j:j + 1], scalar2=None, op0=ALU.mult),
                                          reads=[bacc, bdn], **({"writes": [b_ot]} if ot_state["first"] else {"pwrites": [b_ot]}))
                                    ot_state["first"] = False

                            for (kt, c0, c1, ms, subs) in tiles:
                                jobs.append(make_job(H, KT["a", g][:, kt * 128:(kt + 1) * 128], c0, c1, ms, VV["a", g][:, kt, :], acc, bacc, subs, pv_state, [], fin_swa))
                    run_jobs(jobs)
                    fw.op("dve", lambda e: e.tensor_copy(out=otile[:, :, 1024:2048], in_=ofp[:].rearrange("p j h d -> p j (h d)")), reads=[b_ofp], pwrites=[b_ot])
                    fw.dma("sp", lambda e: e.dma_start(out=otok[q0:q0 + 512, :].rearrange("(j p) c -> p j c", p=128), in_=otile[:]), reads=[b_ot], pwrites=[b_otok])
                fw.barrier()

        def layer_norm_tile(yt, b_yt, gb, bb, b_gb, stats, mv, b_st):
            for c4 in range(4):
                fw.op("dve", lambda e: e.bn_stats(out=stats[:, c4, :], in_=yt[:, c4 * 512:(c4 + 1) * 512]), reads=[b_yt],
                      **({"writes": [b_st]} if c4 == 0 else {"pwrites": [b_st]}))
            fw.op("dve", lambda e: e.bn_aggr(out=mv[:, 0:2], in_=stats[:].rearrange("p a b -> p (a b)")), reads=[b_st], writes=[b_st])
            fw.op("dve", lambda e: e.tensor_scalar(out=mv[:, 2:3], in0=mv[:, 1:2], scalar1=1e-5, scalar2=None, op0=ALU.add), reads=[b_st], writes=[b_st])
            fw.op("act", lambda e: e.activation(out=mv[:, 2:3], in_=mv[:, 2:3], func=AF.Sqrt), reads=[b_st], writes=[b_st])
            fw.op("dve", lambda e: e.reciprocal(out=mv[:, 3:4], in_=mv[:, 2:3]), reads=[b_st], writes=[b_st])
            fw.op("dve", lambda e: e.tensor_scalar(out=yt[:], in0=yt[:], scalar1=mv[:, 0:1], scalar2=mv[:, 3:4], op0=ALU.subtract, op1=ALU.mult),
                  reads=[b_st, b_yt], writes=[b_yt])
            fw.op("pool", lambda e: e.tensor_tensor(out=yt[:], in0=yt[:], in1=gb[:], op=ALU.mult), reads=[b_yt, b_gb], writes=[b_yt])
            fw.op("pool", lambda e: e.tensor_tensor(out=yt[:], in0=yt[:], in1=bb[:], op=ALU.add), reads=[b_yt, b_gb], writes=[b_yt])

        NR = NG + NE

        def phase_outproj_router(l, xsrc, b_xsrc, destI, wts, b_rt):
            with contextlib.ExitStack() as ph:
                wo = sb(ph, "wo", [128, 16, 2048], BF16)
                b_wo = Buf("wo")
                fw.dma("pool", lambda e: e.dma_start(out=wo[:], in_=Wd["w_out"][l].rearrange("(k p) c -> p k c", p=128)), writes=[b_wo])
                bo = sb(ph, "bo", [128, D], F32)
                g1 = sb(ph, "g1", [128, D], F32)
                b1 = sb(ph, "b1", [128, D], F32)
                b_gb = Buf("gb")
                for t_, nm in ((bo, "b_out"), (g1, "ln1_g"), (b1, "ln1_b")):
                    fw.dma("sp", lambda e: e.dma_start(out=t_[:], in_=Wd[nm][l, :].partition_broadcast(128)), pwrites=[b_gb])
                wr = sb(ph, "wr", [128, 16, NR], F32)
                rb = sb(ph, "rb", [128, NR], F32)
                with nc.allow_non_contiguous_dma(reason="small router weight rows"):
                    fw.dma("sp", lambda e: e.dma_start(out=wr[:, :, 0:NG], in_=Wd["w_group"][l].rearrange("(k p) c -> p k c", p=128)), pwrites=[b_gb])
                    fw.dma("sp", lambda e: e.dma_start(out=wr[:, :, NG:NR], in_=Wd["w_expert"][l].rearrange("(k p) c -> p k c", p=128)), pwrites=[b_gb])
                fw.dma("sp", lambda e: e.dma_start(out=rb[:, 0:NG], in_=Wd["b_group"][l, :].partition_broadcast(128)), pwrites=[b_gb])
                fw.dma("sp", lambda e: e.dma_start(out=rb[:, NG:NR], in_=Wd["b_expert"][l, :].partition_broadcast(128)), pwrites=[b_gb])
                ltri = sb(ph, "ltri", [128, 128], F32)
                onesf = sb(ph, "onesf", [128, 128], F32)
                iotae = sb(ph, "iotae", [128, NE], F32)
                fw.dma("sp", lambda e: e.dma_start(out=ltri[:], in_=Cd["ltri"][:, :]), pwrites=[b_gb])
                fw.dma("sp", lambda e: e.dma_start(out=onesf[:], in_=Cd["onesf"][:, :]), pwrites=[b_gb])
                fw.dma("sp", lambda e: e.dma_start(out=iotae[:], in_=Cd["iotae"][:, :]), pwrites=[b_gb])
                ot = [sb(ph, f"ot{i}", [128, D], BF16) for i in range(2)]
                xr = [sb(ph, f"xr{i}", [128, D], F32) for i in range(2)]
                b_ot = [Buf(f"ot{i}") for i in range(2)]
                b_xr = [Buf(f"xr{i}") for i in range(2)]
                oT = sb(ph, "oT", [128, 16, 128], BF16)
                b_oT = Buf("oT")
                yt = [sb(ph, f"yt{i}", [128, D], F32) for i in range(2)]
                b_yt = [Buf(f"yt{i}") for i in range(2)]
                x1b = [sb(ph, f"x1b{i}", [128, D], BF16) for i in range(2)]
                b_x1b = [Buf(f"x1b{i}") for i in range(2)]
                xT1 = sb(ph, "xT1", [128, 16, 128], F32)
                b_xT1 = Buf("xT1")
                stats = sb(ph, "stats", [128, 4, 6], F32)
                mv = sb(ph, "mv", [128, 4], F32)
                b_st = Buf("st")
                lg = sb(ph, "lg", [128, NR], F32)
                sm = sb(ph, "sm", [128, 16], F32)
                ohg = sb(ph, "ohg", [128, NG], F32)
                ge = sb(ph, "ge", [128, NG], F32)
                esel = sb(ph, "esel", [128, 8], F32)
                m8r = sb(ph, "m8r", [128, 8], F32)
                oh = sb(ph, "oh", [128, 2, 8], F32)
                Ak = sb(ph, "Ak", [128, 2, NE], F32)
                Asum = sb(ph, "Asum", [128, NE], F32)
                Acum = sb(ph, "Acum", [128, NE], F32)
                cntt = sb(ph, "cntt", [128, NE], F32)
                tmpe = sb(ph, "tmpe", [128, NE], F32)
                dstf = sb(ph, "dstf", [128, 2], F32)
                tmp2 = sb(ph, "tmp2", [128, 2, NE], F32)
                sm2 = sb(ph, "sm2", [128, 16], F32)
                b_r = Buf("router")
                b_ac = Buf("acum")
                fw.op("dve", lambda e: e.memset(Acum[:], 0.0), writes=[b_ac])
                def stX(ti):
                    s = ti % 2
                    r0 = ti * 128
                    fw.dma("sp", lambda e: e.dma_start(out=ot[s][:], in_=otok[r0:r0 + 128, :]), reads=[b_otok], writes=[b_ot[s]])
                    fw.dma("sp", lambda e: e.dma_start(out=xr[s][:], in_=xsrc[r0:r0 + 128, :]), reads=[b_xsrc], writes=[b_xr[s]])
                    for kq in range(4):
                        bk = kq % 2
                        pb = PS[bk][:, :].bitcast(BF16)
                        for kk in range(4):
                            k = kq * 4 + kk
                            fw.op("pe", lambda e: e.transpose(pb[:, kk * 128:(kk + 1) * 128], ot[s][:, k * 128:(k + 1) * 128], ident_b[:]),
                                  reads=[b_ot[s], bC], **({"writes": [bPS[bk]]} if kk == 0 else {"pwrites": [bPS[bk]]}))
                        fw.op("act", lambda e: e.activation(out=oT[:, kq * 4:(kq + 1) * 4, :], in_=pb[:, 0:512].rearrange("p (k t) -> p k t", k=4), func=AF.Copy),
                              reads=[bPS[bk]], **({"writes": [b_oT]} if kq == 0 else {"pwrites": [b_oT]}))
                    fw.op("pool", lambda e: e.tensor_scalar(out=xr[s][:], in0=xr[s][:], scalar1=float(cfg.ALPHA), scalar2=None, op0=ALU.mult), reads=[b_xr[s]], writes=[b_xr[s]])
                    fw.op("pool", lambda e: e.tensor_tensor(out=xr[s][:], in0=xr[s][:], in1=bo[:], op=ALU.add), reads=[b_xr[s], b_gb], writes=[b_xr[s]])
                    for cb in range(4):
                        for k in range(16):
                            mm(PS[4 + cb][:, :], oT[:, k, :], wo[:, k, cb * 512:(cb + 1) * 512], k == 0, k == 15, [b_oT, b_wo], bPS[4 + cb], k == 0)

                def stX2(ti):
                    s = ti % 2
                    r0 = ti * 128
                    for cb in range(4):
                        fw.op("dve", lambda e: e.tensor_tensor(out=yt[s][:, cb * 512:(cb + 1) * 512], in0=PS[4 + cb][:, :], in1=xr[s][:, cb * 512:(cb + 1) * 512], op=ALU.add),
                              reads=[bPS[4 + cb], b_xr[s]], **({"writes": [b_yt[s]]} if cb == 0 else {"pwrites": [b_yt[s]]}))
                    layer_norm_tile(yt[s], b_yt[s], g1, b1, b_gb, stats, mv, b_st)
                    fw.dma("sp", lambda e: e.dma_start(out=x1d[r0:r0 + 128, :], in_=yt[s][:]), reads=[b_yt[s]], pwrites=[b_x1d])
                    fw.op("act", lambda e: e.activation(out=x1b[s][:], in_=yt[s][:], func=AF.Copy), reads=[b_yt[s]], writes=[b_x1b[s]])
                def stY(ti):
                    s = ti % 2
                    for kq in range(4):
                        bk = 2 + kq % 2
                        for kk in range(4):
                            k = kq * 4 + kk
                            fw.op("pe", lambda e: e.transpose(PS[bk][:, kk * 128:(kk + 1) * 128], yt[s][:, k * 128:(k + 1) * 128], ident_f[:]),
                                  reads=[b_yt[s], bC], **({"writes": [bPS[bk]]} if kk == 0 else {"pwrites": [bPS[bk]]}))
                        fw.op("act", lambda e: e.activation(out=xT1[:, kq * 4:(kq + 1) * 4, :], in_=PS[bk][:, :].rearrange("p (k t) -> p k t", k=4), func=AF.Copy),
                              reads=[bPS[bk]], **({"writes": [b_xT1]} if kq == 0 else {"pwrites": [b_xT1]}))
                    for k in range(16):
                        mm(PS[2][:, 0:NR], xT1[:, k, :], wr[:, k, :], k == 0, k == 15, [b_xT1, b_gb], bPS[2], k == 0)
                    R = [b_r]
                    fw.op("dve", lambda e: e.tensor_tensor(out=lg[:], in0=PS[2][:, 0:NR], in1=rb[:], op=ALU.add), reads=[bPS[2], b_gb], writes=R)
                    fw.op("dve", lambda e: e.tensor_reduce(out=sm[:, 0:1], in_=lg[:, 0:NG], axis=AX.X, op=ALU.max), reads=R, writes=R)
                    fw.op("dve", lambda e: e.tensor_scalar(out=ohg[:], in0=lg[:, 0:NG], scalar1=sm[:, 0:1], scalar2=None, op0=ALU.is_equal), reads=R, writes=R)
                    fw.op("dve", lambda e: e.tensor_scalar(out=sm[:, 1:2], in0=sm[:, 0:1], scalar1=-1.0, scalar2=None, op0=ALU.mult), reads=R, writes=R)
                    fw.op("act", lambda e: e.activation(out=ge[:], in_=lg[:, 0:NG], func=AF.Exp, bias=sm[:, 1:2], scale=1.0), reads=R, writes=R)
                    fw.op("dve", lambda e: e.tensor_reduce(out=sm[:, 2:3], in_=ge[:], axis=AX.X, op=ALU.add), reads=R, writes=R)
                    fw.op("dve", lambda e: e.reciprocal(out=sm[:, 3:4], in_=sm[:, 2:3]), reads=R, writes=R)
                    lge = lg[:, NG:NR].rearrange("p (g e) -> p g e", e=8)
                    fw.op("dve", lambda e: e.tensor_tensor(out=tmpe[:].rearrange("p (g e) -> p g e", e=8), in0=lge, in1=ohg[:, :].unsqueeze(2).to_broadcast([128, NG, 8]), op=ALU.mult),
                          reads=R, writes=R)
                    fw.op("dve", lambda e: e.tensor_reduce(out=esel[:], in_=tmpe[:].rearrange("p (g e) -> p e g", e=8), axis=AX.X, op=ALU.add), reads=R, writes=R)
                    fw.op("dve", lambda e: e.max(out=m8r[:], in_=esel[:]), reads=R, writes=R)
                    fw.op("dve", lambda e: e.tensor_tensor(out=oh[:], in0=esel[:, :].unsqueeze(1).to_broadcast([128, 2, 8]), in1=m8r[:, 0:2].unsqueeze(2).to_broadcast([128, 2, 8]), op=ALU.is_equal),
                          reads=R, writes=R)
                    fw.op("dve", lambda e: e.tensor_tensor(out=sm[:, 4:5], in0=m8r[:, 1:2], in1=m8r[:, 0:1], op=ALU.subtract), reads=R, writes=R)
                    fw.op("act", lambda e: e.activation(out=sm[:, 5:6], in_=sm[:, 4:5], func=AF.Exp), reads=R, writes=R)
                    fw.op("dve", lambda e: e.tensor_scalar(out=sm[:, 6:7], in0=sm[:, 5:6], scalar1=1.0, scalar2=None, op0=ALU.add), reads=R, writes=R)
                    fw.op("dve", lambda e: e.reciprocal(out=sm[:, 6:7], in_=sm[:, 6:7]), reads=R, writes=R)
                    fw.op("dve", lambda e: e.tensor_tensor(out=sm[:, 7:8], in0=sm[:, 6:7], in1=sm[:, 3:4], op=ALU.mult), reads=R, writes=R)
                    fw.op("dve", lambda e: e.tensor_tensor(out=sm[:, 8:9], in0=sm[:, 7:8], in1=sm[:, 5:6], op=ALU.mult), reads=R, writes=R)
                    for k2 in range(2):
                        fw.op("dve", lambda e: e.tensor_tensor(out=Ak[:, k2, :].rearrange("p (g e) -> p g e", e=8), in0=oh[:, k2, :].unsqueeze(1).to_broadcast([128, NG, 8]),
                                                               in1=ohg[:, :].unsqueeze(2).to_broadcast([128, NG, 8]), op=ALU.mult), reads=R, writes=R)
                    fw.op("dve", lambda e: e.tensor_tensor(out=Asum[:], in0=Ak[:, 0, :], in1=Ak[:, 1, :], op=ALU.add), reads=R, writes=R)
                    mm(PS[3][:, 0:NE], ltri[:], Asum[:], True, False, [b_r, b_gb], bPS[3], True)
                    mm(PS[3][:, 0:NE], onesf[:], Acum[:], False, True, [b_ac, b_gb], bPS[3], False)
                    fw.op("dve", lambda e: e.tensor_copy(out=cntt[:], in_=PS[3][:, 0:NE]), reads=[bPS[3]], writes=R)
                    fw.op("dve", lambda e: e.tensor_tensor(out=Acum[:], in0=Acum[:], in1=Asum[:], op=ALU.add), reads=R + [b_ac], writes=[b_ac])
                    fw.op("dve", lambda e: e.tensor_tensor(out=tmp2[:], in0=Ak[:], in1=cntt[:, :].unsqueeze(1).to_broadcast([128, 2, NE]), op=ALU.mult), reads=R, writes=R)
                    fw.op("dve", lambda e: e.tensor_reduce(out=sm2[:, 0:2], in_=tmp2[:], axis=AX.X, op=ALU.add), reads=R, writes=R)
                    fw.op("dve", lambda e: e.tensor_tensor(out=tmp2[:], in0=Ak[:], in1=iotae[:, :].unsqueeze(1).to_broadcast([128, 2, NE]), op=ALU.mult), reads=R + [b_gb], writes=R)
                    fw.op("dve", lambda e: e.tensor_reduce(out=sm2[:, 2:4], in_=tmp2[:], axis=AX.X, op=ALU.add), reads=R, writes=R)
                    fw.op("dve", lambda e: e.tensor_scalar(out=sm2[:, 4:6], in0=sm2[:, 0:2], scalar1=float(CAP), scalar2=None, op0=ALU.is_lt), reads=R, writes=R)
                    fw.op("dve", lambda e: e.scalar_tensor_tensor(out=sm2[:, 6:8], in0=sm2[:, 2:4], scalar=float(CAP), in1=sm2[:, 0:2], op0=ALU.mult, op1=ALU.add), reads=R, writes=R)
                    fw.op("dve", lambda e: e.tensor_scalar(out=sm2[:, 8:10], in0=sm2[:, 4:6], scalar1=-1e6, scalar2=1e6, op0=ALU.mult, op1=ALU.add), reads=R, writes=R)
                    fw.op("dve", lambda e: e.tensor_tensor(out=dstf[:], in0=sm2[:, 6:8], in1=sm2[:, 8:10], op=ALU.add), reads=R, writes=R)
                    fw.op("dve", lambda e: e.tensor_tensor(out=wts[:, ti, :], in0=sm[:, 7:9], in1=sm2[:, 4:6], op=ALU.mult), reads=R, pwrites=[b_rt])
                    fw.op("dve", lambda e: e.tensor_copy(out=destI[:, ti, :], in_=dstf[:]), reads=R, pwrites=[b_rt])
                    for k2 in range(2):
                        fw.dma("pool", lambda e: e.indirect_dma_start(out=xd[:, :], out_offset=bass.IndirectOffsetOnAxis(ap=destI[:, ti, k2:k2 + 1], axis=0),
                                                                      in_=x1b[s][:], in_offset=None, bounds_check=bc_reg, oob_is_err=False),
                               reads=[b_x1b[s], b_rt], pwrites=[b_xd])
                stX(0)
                stX2(0)
                for ti in range(NT):
                    if ti + 1 < NT:
                        stX(ti + 1)
                    stY(ti)
                    if ti + 1 < NT:
                        stX2(ti + 1)
                fw.barrier()

        CT = CAP // 128

        def phase_experts(l):
            with contextlib.ExitStack() as ph:
                wg = [sb(ph, f"wg{i}", [128, 16, 512], BF16) for i in range(2)]
                wu = [sb(ph, f"wu{i}", [128, 16, 512], BF16) for i in range(2)]
                wdn = [sb(ph, f"wdn{i}", [128, 4, 2048], BF16) for i in range(2)]
                b_wgu = [Buf(f"wgu{i}") for i in range(2)]
                b_wd = [Buf(f"wd{i}") for i in range(2)]
                xe = [sb(ph, f"xe{i}", [128, CT, D], BF16) for i in range(2)]
                b_xe = [Buf(f"xe{i}") for i in range(2)]
                xeT = [sb(ph, f"xeT{i}", [128, 16, CAP], BF16) for i in range(2)]
                b_xeT = [Buf(f"xeT{i}") for i in range(2)]
                hT = [sb(ph, f"ehT{i}", [128, 4, CAP], BF16) for i in range(2)]
                b_hT = [Buf(f"ehT{i}") for i in range(2)]
                sgl = [sb(ph, f"sgl{i}", [128, CAP], F32) for i in range(2)]
                b_sgl = [Buf(f"sgl{i}") for i in range(2)]
                ye = [sb(ph, f"ye{i}", [128, D], F32) for i in range(2)]
                b_ye = [Buf(f"ye{i}") for i in range(2)]
                cnt2 = {"sg": 0, "ye": 0}

                def load_gu(e_):
                    if e_ >= NE:
                        return
                    s = e_ % 2
                    fw.dma("pool", lambda e: e.dma_start(out=wg[s][:], in_=Wd["we_gate"][l, e_].rearrange("(k p) c -> p k c", p=128)), writes=[b_wgu[s]])
                    fw.dma("pool", lambda e: e.dma_start(out=wu[s][:], in_=Wd["we_up"][l, e_].rearrange("(k p) c -> p k c", p=128)), pwrites=[b_wgu[s]])

                def load_d(e_):
                    if e_ >= NE:
                        return
                    s = e_ % 2
                    fw.dma("pool", lambda e: e.dma_start(out=wdn[s][:], in_=Wd["we_down"][l, e_].rearrange("(k p) c -> p k c", p=128)), writes=[b_wd[s]])

                def load_x(e_):
                    if e_ >= NE:
                        return
                    s = e_ % 2
                    fw.dma("sp", lambda e: e.dma_start(out=xe[s][:], in_=xd[e_ * CAP:(e_ + 1) * CAP, :].rearrange("(c p) d -> p c d", p=128)), reads=[b_xd], writes=[b_xe[s]])

                def stP(e_):
                    s = e_ % 2
                    for c in range(CT):
                        for kq in range(4):
                            bk = kq % 2
                            pbk = PS[bk][:, :].bitcast(BF16)
                            for kk in range(4):
                                k = kq * 4 + kk
                                fw.op("pe", lambda e: e.transpose(pbk[:, kk * 128:(kk + 1) * 128], xe[s][:, c, k * 128:(k + 1) * 128], ident_b[:]),
                                      reads=[b_xe[s], bC], **({"writes": [bPS[bk]]} if kk == 0 else {"pwrites": [bPS[bk]]}))
                            first = (c == 0 and kq == 0)
                            dst = xeT[s][:, kq * 4:(kq + 1) * 4, c * 128:(c + 1) * 128]
                            srcp = pbk[:, 0:512].rearrange("p (k t) -> p k t", k=4)
                            if kq % 2 == 0:
                                fw.op("act", lambda e: e.activation(out=dst, in_=srcp, func=AF.Copy), reads=[bPS[bk]], **({"writes": [b_xeT[s]]} if first else {"pwrites": [b_xeT[s]]}))
                            else:
                                fw.op("dve", lambda e: e.tensor_copy(out=dst, in_=srcp), reads=[bPS[bk]], **({"writes": [b_xeT[s]]} if first else {"pwrites": [b_xeT[s]]}))

                def stQ(e_):
                    s = e_ % 2
                    for fc in range(4):
                        bg = 2 + (fc % 2) * 2
                        bu = bg + 1
                        for k in range(16):
                            mm(PS[bg][:, 0:CAP], wg[s][:, k, fc * 128:(fc + 1) * 128], xeT[s][:, k, :], k == 0, k == 15, [b_wgu[s], b_xeT[s]], bPS[bg], k == 0)
                        for k in range(16):
                            mm(PS[bu][:, 0:CAP], wu[s][:, k, fc * 128:(fc + 1) * 128], xeT[s][:, k, :], k == 0, k == 15, [b_wgu[s], b_xeT[s]], bPS[bu], k == 0)
                        q = cnt2["sg"] % 2
                        cnt2["sg"] += 1
                        fw.op("act", lambda e: e.activation(out=sgl[q][:], in_=PS[bg][:, 0:CAP], func=AF.Silu), reads=[bPS[bg]], writes=[b_sgl[q]])
                        fw.op("dve", lambda e: e.tensor_tensor(out=hT[s][:, fc, :], in0=sgl[q][:], in1=PS[bu][:, 0:CAP], op=ALU.mult), reads=[b_sgl[q], bPS[bu]],
                              **({"writes": [b_hT[s]]} if fc == 0 else {"pwrites": [b_hT[s]]}))

                def stR(e_):
                    s = e_ % 2
                    for c in range(CT):
                        q = cnt2["ye"] % 2
                        cnt2["ye"] += 1
                        for cb in range(4):
                            bk = 6 + cb % 2
                            for fc in range(4):
                                mm(PS[bk][:, :], hT[s][:, fc, c * 128:(c + 1) * 128], wdn[s][:, fc, cb * 512:(cb + 1) * 512], fc == 0, fc == 3, [b_hT[s], b_wd[s]], bPS[bk], fc == 0)
                            if cb % 2 == 0:
                                fw.op("act", lambda e: e.activation(out=ye[q][:, cb * 512:(cb + 1) * 512], in_=PS[bk][:, :], func=AF.Copy), reads=[bPS[bk]],
                                      **({"writes": [b_ye[q]]} if cb == 0 else {"pwrites": [b_ye[q]]}))
                            else:
                                fw.op("dve", lambda e: e.tensor_copy(out=ye[q][:, cb * 512:(cb + 1) * 512], in_=PS[bk][:, :]), reads=[bPS[bk]], pwrites=[b_ye[q]])
                        r0 = e_ * CAP + c * 128
                        fw.dma("sp", lambda e: e.dma_start(out=yd[r0:r0 + 128, :], in_=ye[q][:]), reads=[b_ye[q]], pwrites=[b_yd])

                load_x(0)
                load_gu(0)
                load_d(0)
                load_x(1)
                load_gu(1)
                load_d(1)
                for it in range(NE + 2):
                    if it < NE:
                        stP(it)
                        load_x(it + 2)
                    if 0 <= it - 1 < NE:
                        stQ(it - 1)
                        if it + 1 >= 2:
                            load_gu(it + 1)
                    if 0 <= it - 2 < NE:
                        stR(it - 2)
                        load_d(it)
                fw.barrier()

        def phase_combine(l, destI, wts, b_rt, dst, b_dst):
            with contextlib.ExitStack() as ph:
                g2 = sb(ph, "g2", [128, D], F32)
                b2 = sb(ph, "b2", [128, D], F32)
                b_gb = Buf("gb2")
                for t_, nm in ((g2, "ln2_g"), (b2, "ln2_b")):
                    fw.dma("sp", lambda e: e.dma_start(out=t_[:], in_=Wd[nm][l, :].partition_broadcast(128)), pwrites=[b_gb])
                x1t = [sb(ph, f"x1t{i}", [128, D], F32) for i in range(2)]
                b_x1t = [Buf(f"x1t{i}") for i in range(2)]
                rr = [[sb(ph, f"rr{i}{k}", [128, D], F32) for k in range(2)] for i in range(2)]
                b_rr = [[Buf(f"rr{i}{k}") for k in range(2)] for i in range(2)]
                stats = sb(ph, "stats2", [128, 4, 6], F32)
                mv = sb(ph, "mv2", [128, 4], F32)
                b_st = Buf("st2")
                for i in range(2):
                    for k in range(2):
                        fw.op("pool", lambda e: e.memset(rr[i][k][:], 0.0), writes=[b_rr[i][k]])
                def g_loads(ti):
                    s = ti % 2
                    r0 = ti * 128
                    fw.dma("sp", lambda e: e.dma_start(out=x1t[s][:], in_=x1d[r0:r0 + 128, :]), reads=[b_x1d], writes=[b_x1t[s]])
                    for k2 in range(2):
                        fw.dma("pool", lambda e: e.indirect_dma_start(out=rr[s][k2][:], out_offset=None, in_=yd[:, :],
                                                                      in_offset=bass.IndirectOffsetOnAxis(ap=destI[:, ti, k2:k2 + 1], axis=0),
                                                                      bounds_check=bc_reg, oob_is_err=False),
                               reads=[b_yd, b_rt], writes=[b_rr[s][k2]])

                g_loads(0)
                for ti in range(NT):
                    s = ti % 2
                    r0 = ti * 128
                    if ti + 1 < NT:
                        g_loads(ti + 1)
                    fw.op("act", lambda e: e.activation(out=x1t[s][:], in_=x1t[s][:], func=AF.Copy, scale=float(cfg.ALPHA)), reads=[b_x1t[s]], writes=[b_x1t[s]])
                    for k2 in range(2):
                        fw.op("dve", lambda e: e.scalar_tensor_tensor(out=x1t[s][:], in0=rr[s][k2][:], scalar=wts[:, ti, k2:k2 + 1], in1=x1t[s][:], op0=ALU.mult, op1=ALU.add),
                              reads=[b_rr[s][k2], b_rt, b_x1t[s]], writes=[b_x1t[s]])
                    layer_norm_tile(x1t[s], b_x1t[s], g2, b2, b_gb, stats, mv, b_st)
                    fw.dma("sp", lambda e: e.dma_start(out=dst[r0:r0 + 128, :], in_=x1t[s][:]), reads=[b_x1t[s]], pwrites=[b_dst])
                fw.barrier()

        xsrc, b_xsrc = x_in, b_xin
        for l in range(cfg.DEPTH):
            phase_inproj(l, xsrc, b_xsrc)
            if stop_after == "A":
                break
            phase_attention(l)
            if stop_after == "C":
                break
            with contextlib.ExitStack() as lay:
                destI = sb(lay, "destI", [128, NT, 2], I32)
                wts = sb(lay, "wts", [128, NT, 2], F32)
                b_rt = Buf("route")
                phase_outproj_router(l, xsrc, b_xsrc, destI, wts, b_rt)
                if stop_after == "D":
                    break
                phase_experts(l)
                if stop_after == "F":
                    break
                last = (l == cfg.DEPTH - 1)
                dst, b_dst = (y_out, b_yout) if last else (xmid, b_xmid)
                phase_combine(l, destI, wts, b_rt, dst, b_dst)
            xsrc, b_xsrc = xmid, b_xmid
        fw.barrier()
    return nc, hc, dbg, fw.ninstr


_CACHE = {}


def kernel(**inputs):
    cfg = Cfg(S=4096, DEPTH=2, NG=8, CAP=256)
    if "nc" not in _CACHE:
        _CACHE["nc"] = build(cfg)
    nc, hc, _, _ = _CACHE["nc"]
    x = np.asarray(inputs["x"], np.float32)
    B = x.shape[0]
    base = {}
    for k in WEIGHT_SHAPES(cfg):
        base[k] = np.ascontiguousarray(np.asarray(inputs[k], np.float32))
    for k, v in hc.items():
        base["c_" + k] = v
    in_maps = []
    for c in range(8):
        m = dict(base)
        m["x"] = np.ascontiguousarray(x[c % B])
        in_maps.append(m)
    res = run_bass_kernel_spmd(nc, in_maps, core_ids=list(range(8)))
    out = np.stack([np.asarray(res.results[b]["y"], np.float32) for b in range(B)], axis=0)
    return out
```
